# Optimizing a Trainium2 kernel written in Bass

```python
import jax, jax.numpy as jnp
from jax import lax
import numpy as np

D_MODEL = 1024
BATCH = 4
SEQ = 8192
DEPTH = 2

N_MEM = 256
POOL_WINDOWS = (2, 4, 8, 16)
POOL_GROUPS = 4
POOL_GROUP_DIM = D_MODEL // 8
POOL_WIDTH = POOL_GROUPS * POOL_GROUP_DIM
RET_HEADS = 4
RET_DK = D_MODEL // 8
RET_DV = D_MODEL // 4
RET_QK_WIDTH = RET_HEADS * RET_DK
RET_V_WIDTH = RET_HEADS * RET_DV
RET_CHUNK = 128
ROPE_BASE = 10000.0
XATTN_HEADS = 4
XATTN_DH = D_MODEL // 8
XATTN_WIDTH = XATTN_HEADS * XATTN_DH
N_BRANCHES = 3
D_FF = 4 * D_MODEL
NORM_EPS = 1e-6
IN_SPLITS = (POOL_WIDTH, RET_QK_WIDTH, RET_QK_WIDTH, RET_V_WIDTH, RET_V_WIDTH, XATTN_WIDTH, N_BRANCHES * D_MODEL)
IN_COLS = sum(IN_SPLITS)
IN_OFFSETS = tuple(int(v) for v in np.cumsum(IN_SPLITS)[:-1])

kernel_name = "gated_pool_retention_memory_hybrid"


def rms_norm(x, g):
    xf = x.astype(jnp.float32)
    y = xf * lax.rsqrt(jnp.mean(xf * xf, axis=-1, keepdims=True) + NORM_EPS)
    return (y * g.astype(jnp.float32)).astype(x.dtype)


def pool_mixer(u, pool_w, pool_scale):
    B, S, _ = u.shape
    uf = u.astype(jnp.float32).reshape(B, S, POOL_GROUPS, POOL_GROUP_DIM)
    csum = jnp.cumsum(uf, axis=1)
    t = jnp.arange(S)
    outs = []
    for gi, w in enumerate(POOL_WINDOWS):
        cg = csum[:, :, gi]
        shifted = jnp.pad(cg, ((0, 0), (w, 0), (0, 0)))[:, :S]
        cnt = jnp.minimum(t + 1, w).astype(jnp.float32)[None, :, None]
        outs.append((cg - shifted) / cnt - uf[:, :, gi])
    d = jnp.stack(outs, axis=2).astype(u.dtype)
    y = jnp.einsum('bsgc,gcd->bsgd', d, pool_w).reshape(B, S, POOL_WIDTH)
    return y * pool_scale


def rotary(x, positions):
    half = x.shape[-1] // 2
    inv_freq = ROPE_BASE ** (-jnp.arange(half, dtype=jnp.float32) / half)
    ang = positions.astype(jnp.float32)[:, :, None] * inv_freq
    cos = jnp.cos(ang)[:, :, None, :]
    sin = jnp.sin(ang)[:, :, None, :]
    x1, x2 = x[..., :half], x[..., half:]
    return jnp.concatenate([x1 * cos - x2 * sin, x2 * cos + x1 * sin], axis=-1)


def retention(q, k, v, gate, positions, ret_norm_g):
    B, S, _ = q.shape
    dt = v.dtype
    N, C, H = S // RET_CHUNK, RET_CHUNK, RET_HEADS
    qf = rotary(q.astype(jnp.float32).reshape(B, S, H, RET_DK), positions)
    kf = rotary(k.astype(jnp.float32).reshape(B, S, H, RET_DK), positions) * (RET_DK ** -0.5)
    vf = v.astype(jnp.float32).reshape(B, S, H, RET_DV)
    qc = qf.reshape(B, N, C, H, RET_DK)
    kc = kf.reshape(B, N, C, H, RET_DK)
    vc = vf.reshape(B, N, C, H, RET_DV)

    log_gamma = jnp.log(1.0 - 2.0 ** (-5.0 - jnp.arange(H, dtype=jnp.float32)))
    idx = jnp.arange(C)
    diff = (idx[:, None] - idx[None, :]).astype(jnp.float32)
    causal = idx[:, None] >= idx[None, :]
    decay_mask = jnp.where(causal[None], jnp.exp(diff[None] * log_gamma[:, None, None]), 0.0)

    scores = jnp.einsum('bnihd,bnjhd->bnhij', qc, kc) * decay_mask[None, None]
    y_intra = jnp.einsum('bnhij,bnjhv->bnihv', scores, vc)

    idx_f = idx.astype(jnp.float32)
    k_dec = kc * jnp.exp((C - 1.0 - idx_f)[:, None] * log_gamma[None, :])[..., None]
    kv = jnp.einsum('bnjhd,bnjhv->bnhdv', k_dec, vc)
    chunk_decay = jnp.exp(C * log_gamma)[None, :, None, None]

    def step(state, kv_n):
        return chunk_decay * state + kv_n, state

    init = jnp.zeros((B, H, RET_DK, RET_DV), jnp.float32)
    _, s_prev = lax.scan(step, init, jnp.moveaxis(kv, 1, 0))
    s_prev = jnp.moveaxis(s_prev, 0, 1)
    q_dec = qc * jnp.exp((idx_f + 1.0)[:, None] * log_gamma[None, :])[..., None]
    y_inter = jnp.einsum('bnihd,bnhdv->bnihv', q_dec, s_prev)

    y = (y_intra + y_inter).reshape(B, S, H, RET_DV)
    y = y * lax.rsqrt(jnp.mean(y * y, axis=-1, keepdims=True) + NORM_EPS)
    y = y.reshape(B, S, RET_V_WIDTH) * ret_norm_g.astype(jnp.float32)
    return (jax.nn.silu(gate.astype(jnp.float32)) * y).astype(dt)


def memory_attention(q, mem_n, w_mem_kv):
    B, S, _ = q.shape
    M = mem_n.shape[1]
    kvm = mem_n @ w_mem_kv
    km = kvm[..., :XATTN_WIDTH].reshape(B, M, XATTN_HEADS, XATTN_DH)
    vm = kvm[..., XATTN_WIDTH:].reshape(B, M, XATTN_HEADS, XATTN_DH)
    qh = q.reshape(B, S, XATTN_HEADS, XATTN_DH)
    s = jnp.einsum('bshd,bmhd->bhsm', qh, km).astype(jnp.float32) * (XATTN_DH ** -0.5)
    p = jax.nn.softmax(s, axis=-1).astype(vm.dtype)
    return jnp.einsum('bhsm,bmhd->bshd', p, vm).reshape(B, S, XATTN_WIDTH)


def setup_inputs(seed: int = 0) -> dict:
    key = jax.random.key(seed)
    ks = jax.random.split(key, 20)
    f32 = jnp.float32

    def w(k, shape, fan_in):
        return jax.random.normal(k, shape, f32) * (fan_in ** -0.5)

    def gain(k, shape):
        return 1.0 + 0.02 * jax.random.normal(k, shape, f32)

    x = jax.random.normal(ks[0], (BATCH, SEQ, D_MODEL), f32)
    mem = jax.random.normal(ks[1], (BATCH, N_MEM, D_MODEL), f32)
    offsets = jax.random.randint(ks[2], (BATCH, 1), 0, 4096, dtype=jnp.int32)
    positions = (offsets + jnp.arange(SEQ, dtype=jnp.int32)[None, :]).astype(jnp.int32)
    return {
        "x": x,
        "mem": mem,
        "positions": positions,
        "norm_mix_g": gain(ks[3], (DEPTH, D_MODEL)),
        "w_in": w(ks[4], (DEPTH, D_MODEL, IN_COLS), D_MODEL),
        "pool_w": w(ks[5], (DEPTH, POOL_GROUPS, POOL_GROUP_DIM, POOL_GROUP_DIM), POOL_GROUP_DIM),
        "pool_scale": gain(ks[6], (DEPTH, POOL_WIDTH)),
        "ret_norm_g": gain(ks[7], (DEPTH, RET_V_WIDTH)),
        "mem_norm_g": gain(ks[8], (DEPTH, D_MODEL)),
        "w_mem_kv": w(ks[9], (DEPTH, D_MODEL, 2 * XATTN_WIDTH), D_MODEL),
        "w_up_pool": w(ks[10], (DEPTH, POOL_WIDTH, D_MODEL), POOL_WIDTH),
        "w_up_ret": w(ks[11], (DEPTH, RET_V_WIDTH, D_MODEL), RET_V_WIDTH),
        "w_up_mem": w(ks[12], (DEPTH, XATTN_WIDTH, D_MODEL), XATTN_WIDTH),
        "w_out": w(ks[13], (DEPTH, D_MODEL, D_MODEL), D_MODEL),
        "norm_mlp_g": gain(ks[14], (DEPTH, D_MODEL)),
        "w_mlp1": w(ks[15], (DEPTH, D_MODEL, D_FF), D_MODEL),
        "w_mlp2": w(ks[16], (DEPTH, D_FF, D_MODEL), D_FF),
        "final_norm_g": gain(ks[17], (D_MODEL,)),
    }


def reference(x, mem, positions, norm_mix_g, w_in, pool_w, pool_scale, ret_norm_g, mem_norm_g,
              w_mem_kv, w_up_pool, w_up_ret, w_up_mem, w_out, norm_mlp_g, w_mlp1, w_mlp2,
              final_norm_g):
    for l in range(DEPTH):
        h = rms_norm(x, norm_mix_g[l])
        proj = h @ w_in[l]
        pool_u, rq, rk, rv, rg, xq, gates = jnp.split(proj, IN_OFFSETS, axis=-1)
        y_pool = pool_mixer(pool_u, pool_w[l], pool_scale[l])
        y_ret = retention(rq, rk, rv, rg, positions, ret_norm_g[l])
        y_mem = memory_attention(xq, rms_norm(mem, mem_norm_g[l]), w_mem_kv[l])
        g_pool, g_ret, g_mem = jnp.split(jax.nn.sigmoid(gates), N_BRANCHES, axis=-1)
        merged = (g_pool * (y_pool @ w_up_pool[l])
                  + g_ret * (y_ret @ w_up_ret[l])
                  + g_mem * (y_mem @ w_up_mem[l]))
        x = x + merged @ w_out[l]
        h2 = rms_norm(x, norm_mlp_g[l])
        x = x + jnp.square(jax.nn.relu(h2 @ w_mlp1[l])) @ w_mlp2[l]
    return rms_norm(x, final_norm_g)
```

```python
import math
import numpy as np
import concourse.bass as bass
import concourse.mybir as mybir
from concourse.bass_utils import run_bass_kernel_spmd

F32 = mybir.dt.float32
BF16 = mybir.dt.bfloat16
I32 = mybir.dt.int32
AF = mybir.ActivationFunctionType
ALU = mybir.AluOpType

NCORES = 8
D = 1024
TOK = 4096
T = 512
NT = TOK // T
NST = 4
H = 4
IN_COLS = 7168
OFF_POOL, OFF_Q, OFF_K, OFF_V, OFF_G, OFF_XQ, OFF_GATE = 0, 512, 1024, 1536, 2560, 3584, 4096
EPS = 1e-6
GAMMA = [1.0 - 2.0 ** (-5.0 - h) for h in range(H)]
GC = [g ** 128 for g in GAMMA]
NBLK = 38
RING = 4
MAGIC = 12582912.0
TWO_PI = 2.0 * math.pi
C1 = 6.28125
C2 = TWO_PI - C1
EXN = 1024 + 64

c_ident = 0
c_cmask = c_ident + 128
c_kscale = c_cmask + 512
c_yscale = c_kscale + 512
c_yscale2 = c_yscale + 4
c_invf = c_yscale2 + 4
c_invcnt = c_invf + 64
c_isB = c_invcnt + 64
c_neghalf = c_isB + 1
c_gcb = c_neghalf + 8
CST_N = c_gcb + 4
V_GMIX, V_GMLP, V_GMEM, V_PSC = 0, 16, 32, 48
VEC_N = 56
R_RETG, R_FING = 0, 2048
ROW_N = 3072


class V:
    __slots__ = ("ap", "key", "lo", "hi")

    def __init__(self, handle, off, dims, esz, pdim=None, track=True):
        if pdim is not None:
            F, p0, np_ = pdim
            self.ap = bass.AP(handle, p0 * F + off, [[F, np_]] + [[s, n] for s, n in dims])
        else:
            self.ap = bass.AP(handle, off, [[s, n] for s, n in dims])
        lo = off + sum(min(0, s * (n - 1)) for s, n in dims)
        hi = off + sum(max(0, s * (n - 1)) for s, n in dims) + 1
        self.key = handle.name if track else None
        self.lo, self.hi = lo * esz, hi * esz


class Arena:
    def __init__(self, nc, name, nelem, dtype, esz, psum=False):
        self.F = nelem
        self.esz = esz
        self.h = (nc.alloc_psum_tensor if psum else nc.alloc_sbuf_tensor)(name, [128, nelem], dtype)
        self.ptr = 0

    def alloc(self, n):
        o = self.ptr
        self.ptr += n
        assert self.ptr <= self.F, (self.h.name, self.ptr, self.F)
        return o

    def v(self, off, dims, p0=0, np_=128):
        return V(self.h, off, dims, self.esz, pdim=(self.F, p0, np_))


class Tl:
    def __init__(self, arena, off, shape):
        self.a, self.off, self.shape = arena, off, tuple(shape)
        st = []
        acc = 1
        for n in reversed(self.shape):
            st.append(acc)
            acc *= n
        self.strides = tuple(reversed(st))
        self.size = acc

    def __getitem__(self, idx):
        if not isinstance(idx, tuple):
            idx = (idx,)
        off = self.off
        dims = []
        for i, n in enumerate(self.shape):
            s = self.strides[i]
            ix = idx[i] if i < len(idx) else slice(None)
            if isinstance(ix, int):
                off += ix * s
            else:
                a = ix.start or 0
                b = n if ix.stop is None else ix.stop
                off += a * s
                dims.append((s, b - a))
        merged = []
        for s, n in dims:
            if merged and merged[-1][0] == s * n:
                merged[-1] = (s, merged[-1][1] * n)
            else:
                merged.append((s, n))
        if not merged:
            merged = [(1, 1)]
        return self.a.v(off, merged)

    def all(self):
        return self[tuple(slice(None) for _ in self.shape)]


class Op:
    __slots__ = ("eng", "fn", "deps", "dma", "sig", "needed")

    def __init__(self, eng, fn, dma):
        self.eng, self.fn, self.dma = eng, fn, dma
        self.deps = set()
        self.sig = None
        self.needed = False


class Sched:
    ENGS = ("pe", "act", "dve", "pool", "sp")

    def __init__(self, ndma=8):
        self.ops = {e: [] for e in self.ENGS}
        self.track = {}
        self.ndma = ndma
        self.dma_ops = {e: [] for e in self.ENGS}

    def _access(self, v, op, is_write):
        if v.key is None:
            return
        segs = self.track.get(v.key, [])
        lo, hi = v.lo, v.hi
        if is_write and op.eng == "pe" and v.key[:2] in ("ps", "pb"):
            lo, hi = 0, 2048
        out = []
        cover = []
        for sg in segs:
            slo, shi, w, rs = sg
            if shi <= lo or slo >= hi:
                out.append(sg)
                continue
            if slo < lo:
                out.append([slo, lo, w, list(rs)])
            if shi > hi:
                out.append([hi, shi, w, list(rs)])
            cover.append([max(slo, lo), min(shi, hi), w, rs])
        for c in cover:
            if c[2] is not None:
                self._dep(op, c[2], raw=not is_write)
            if is_write:
                for r in c[3]:
                    self._dep(op, r, raw=False)
        if is_write:
            out.append([lo, hi, op, []])
        else:
            cover.sort(key=lambda c: c[0])
            pos = lo
            for c in cover:
                if c[0] > pos:
                    out.append([pos, c[0], None, [op]])
                out.append([c[0], c[1], c[2], list(c[3]) + [op]])
                pos = c[1]
            if pos < hi:
                out.append([pos, hi, None, [op]])
        self.track[v.key] = out

    def _dep(self, op, other, raw):
        if other is op:
            return
        if other.eng == op.eng == "pe" and not other.dma and not op.dma:
            return
        op.deps.add(other)

    def add(self, eng, fn, reads=(), writes=(), dma=False, cc=False):
        op = Op(eng, fn, dma or cc)
        self.cc_ops = getattr(self, "cc_ops", [])
        for v in reads:
            self._access(v, op, False)
        for v in writes:
            self._access(v, op, True)
        if cc:
            self.cc_ops.append(op)
        if dma:
            lst = self.dma_ops[eng]
            if len(lst) >= self.ndma:
                op.deps.add(lst[len(lst) - self.ndma])
            lst.append(op)
        self.ops[eng].append(op)
        return op

    def emit(self, nc):
        for e in self.ENGS:
            for op in self.ops[e]:
                for d in op.deps:
                    d.needed = True
        import contextlib
        with contextlib.ExitStack() as es:
            esem = {e: es.enter_context(nc.semaphore("s_" + e)) for e in self.ENGS}
            dsem = {e: [es.enter_context(nc.semaphore("d_%s%d" % (e, i))) for i in range(self.ndma)]
                    for e in self.ENGS if self.dma_ops[e]}
            ccs = getattr(self, "cc_ops", [])
            ccsem = es.enter_context(nc.semaphore("s_cc")) if ccs else None
            for i, op in enumerate(ccs):
                op.sig = (ccsem, i + 1, 1)
            for e in self.ENGS:
                cnt = 0
                dcnt = 0
                for op in self.ops[e]:
                    if op.sig is not None:
                        continue
                    if op.dma:
                        k = dcnt % self.ndma
                        op.sig = (dsem[e][k], 16 * (dcnt // self.ndma + 1), 16)
                        dcnt += 1
                    elif op.needed:
                        cnt += 1
                        op.sig = (esem[e], cnt, 1)
            block = es.enter_context(nc.Block())
            engmap = {"pe": block.tensor, "act": block.scalar, "dve": block.vector, "pool": block.gpsimd,
                      "sp": block.sync}
            final_dma = [op for e in self.ENGS for op in self.dma_ops[e]]
            for e in self.ENGS:
                ops = self.ops[e]
                last = e == "sp"

                def body(eng, ops=ops, last=last):
                    waited = {}
                    for op in ops:
                        for d in sorted(op.deps, key=lambda d: d.sig[1]):
                            sem, val, _ = d.sig
                            if waited.get(id(sem), 0) < val:
                                eng.wait_ge(sem, val)
                                waited[id(sem)] = val
                        ins = op.fn(eng)
                        if op.sig is not None:
                            ins.then_inc(op.sig[0], op.sig[2])
                    if last:
                        for q in self.ENGS:
                            lst = self.dma_ops[q]
                            for op in lst[-self.ndma:]:
                                sem, val, _ = op.sig
                                if waited.get(id(sem), 0) < val:
                                    eng.wait_ge(sem, val)
                                    waited[id(sem)] = val

                engmap[e](body)


class DR:
    def __init__(self, nc, name, shape, dtype, kind, esz, track=True):
        self.h = nc.dram_tensor(name, list(shape), dtype, kind=kind)
        self.esz = esz
        self.track = track

    def v(self, off, dims):
        return V(self.h, off, dims, self.esz, pdim=None, track=self.track)


class _Stop(Exception):
    pass


def build(mode="fused", stage=0, debug=None):
    nc = bass.Bass("TRN2", target_bir_lowering=False)
    fused = mode == "fused"
    S = Sched()
    IN, OUT, INT = "ExternalInput", "ExternalOutput", "Internal"

    do_pre0 = fused or stage == 0
    do_main0 = fused or stage == 1
    do_pre1 = fused or stage == 1
    do_main1 = fused or stage == 2

    x_in = DR(nc, "x", [TOK, D], F32, IN, 4, track=False)
    mem_in = DR(nc, "mem", [256, D], F32, IN, 4, track=False)
    posT = DR(nc, "posT", [128, 32], I32, IN, 4, track=False)
    cst = DR(nc, "cst", [128, CST_N], F32, IN, 4, track=False)
    vecs = DR(nc, "vecs", [128, VEC_N], F32, IN, 4, track=False)
    rows = DR(nc, "rows", [ROW_N], F32, IN, 4, track=False)
    w_in = DR(nc, "w_in", [2, D, IN_COLS], F32, IN, 4, track=False)
    pool_w = DR(nc, "pool_w", [2, 4, 128, 128], F32, IN, 4, track=False)
    w_mem_kv = DR(nc, "w_mem_kv", [2, D, 1024], F32, IN, 4, track=False)
    w_up_pool = DR(nc, "w_up_pool", [2, 512, D], F32, IN, 4, track=False)
    w_up_ret = DR(nc, "w_up_ret", [2, 1024, D], F32, IN, 4, track=False)
    w_up_mem = DR(nc, "w_up_mem", [2, 512, D], F32, IN, 4, track=False)
    w_out = DR(nc, "w_out", [2, D, D], F32, IN, 4, track=False)
    w_mlp1 = DR(nc, "w_mlp1", [2, D, 4096], F32, IN, 4, track=False)
    w_mlp2 = DR(nc, "w_mlp2", [2, 4096, D], F32, IN, 4, track=False)
    wq = DR(nc, "wq", [2, NBLK, 128, 4096], BF16, INT, 2)
    if fused:
        out_d = DR(nc, "out", [TOK, D], F32, OUT, 4)
        xs = DR(nc, "xs", [TOK, D], F32, INT, 4)
        exs = [DR(nc, "exs%d" % l, [128, EXN], F32, INT, 4) for l in range(2)]
        exg = [DR(nc, "exg%d" % l, [256, EXN], F32, INT, 4) for l in range(2)]
    else:
        out_d = DR(nc, "out", [TOK, D], F32, OUT, 4) if stage == 2 else None
        xs = DR(nc, "xs", [TOK, D], F32, OUT if stage == 1 else IN, 4, track=(stage == 1)) if stage >= 1 else None
        exs = [None, None]
        exg = [None, None]
        if stage == 0:
            exs[0] = DR(nc, "exs0", [128, EXN], F32, OUT, 4)
        if stage == 1:
            exg[0] = DR(nc, "exg0", [128, EXN], F32, IN, 4, track=False)
            exs[1] = DR(nc, "exs1", [128, EXN], F32, OUT, 4)
        if stage == 2:
            exg[1] = DR(nc, "exg1", [128, EXN], F32, IN, 4, track=False)
    dbg = DR(nc, "dbg", [128, 4096], F32, OUT, 4) if debug else None

    A32 = Arena(nc, "a32", 21700, F32, 4)
    A16 = Arena(nc, "a16", 58200, BF16, 2)
    PS = [Arena(nc, "ps%d" % i, 512, F32, 4, psum=True) for i in range(4)]
    PB = [Arena(nc, "pb%d" % i, 1024, BF16, 2, psum=True) for i in range(4)]
    pos_h = nc.alloc_sbuf_tensor("pos_i", [128, 32], I32)

    def t32(shape):
        return Tl(A32, A32.alloc(int(np.prod(shape))), shape)

    def t16(shape):
        return Tl(A16, A16.alloc(int(np.prod(shape))), shape)

    cosT = t32([32, 64])
    sinT = t32([32, 64])
    CST = t32([CST_N])
    VEC = t32([VEC_N])
    retg = t32([1024])
    fing = t32([1024])
    Z = t32([H, 256])
    Xb = [t32([NST, 1024]), t32([NST, 1024])]

    class _XRef:
        cur = Xb[0]
        n = 0

        def __getitem__(self, idx):
            return self.cur[idx]

        def flip(self):
            _XRef.n += 1
            _XRef.cur = Xb[_XRef.n % 2]
            return _XRef.cur
    X = _XRef()
    ss = t32([8])
    rstd = t32([8])
    ys = t32([8])
    sc = t32([8])
    halo = t32([4, 16])
    tr32 = A32.ptr
    U = t32([4, 528])
    tmpA = t32([528])
    tmpB = t32([528])
    mtmp = t32([2, 512])
    rsum = t32([512])
    end_a = A32.ptr
    A32.ptr = tr32
    rotA = t32([512])
    rotB = t32([512])
    rotR = t32([512])
    posf = t32([32])
    A32.ptr = max(A32.ptr, end_a)
    exl = Tl(A32, tmpA.off, [EXN])
    ang = Tl(A32, Xb[0].off, [32, 64])
    kk = Tl(A32, Xb[0].off + 2048, [32, 64])
    ident = t16([128])
    ones = t16([128])
    Sbs = [t16([H, 256]) for _ in range(NST)]
    kmT = t16([H, 256])
    vm = t16([2, 512])
    poolw = t16([4, 128])
    ring = [t16([8, 512]) for _ in range(RING)]
    hT = t16([8, 512])
    hn = t16([NST, 1024])
    junk = t16([1024])
    tr16 = A16.ptr
    mergedT = t16([8, 512])
    yretT = t16([8, 512])
    ph = A16.ptr
    qrot = t16([NST, 512])
    kpr = t16([NST, 512])
    vv = t16([NST, 1024])
    gs = t16([NST, 1024])
    qT = [t16([H, 128]) for _ in range(2)]
    kT = [t16([H, 128]) for _ in range(2)]
    scm = [t16([H, 128]) for _ in range(2)]
    yret = [t16([1024]) for _ in range(2)]
    end_r = A16.ptr
    A16.ptr = ph
    dd = t16([4, 512])
    ypoolT = t16([4, 512])
    xqT = t16([4, 512])
    pT = [t16([2, 512]) for _ in range(2)]
    ymemT = t16([4, 512])
    sg = t16([3, 4, 512])
    end_p = A16.ptr
    A16.ptr = tr16 + 4096
    A16.ptr = tr16
    hid = t16([32, 512])
    memn = Tl(A16, hid.off, [2, 1024])
    memnT = Tl(A16, hid.off + 2048, [8, 256])
    A16.ptr = max(end_r, end_p, A16.ptr)

    PE, ACT, DVE, POOL, SP = "pe", "act", "dve", "pool", "sp"

    def mm(out, lhsT, rhs, start=True, stop=True):
        return S.add(PE, lambda e: e.matmul(out.ap, lhsT.ap, rhs.ap, start=start, stop=stop), [lhsT, rhs], [out])

    idv = ident.all()

    def tp(out, in_):
        return S.add(PE, lambda e: e.transpose(out.ap, in_.ap, idv.ap), [in_, idv], [out])

    def act(out, in_, func, scale=None, bias=None, accum=None):
        rd = [in_]
        kw = {}
        for nm, val in (("scale", scale), ("bias", bias)):
            if val is not None:
                if isinstance(val, V):
                    rd.append(val)
                    kw[nm] = val.ap
                else:
                    kw[nm] = float(val)
        wr = [out]
        if accum is not None:
            wr.append(accum)
            kw["accum_out"] = accum.ap
        return S.add(ACT, lambda e: e.activation(out.ap, in_.ap, func, **kw), rd, wr)

    def tt(out, in0, in1, op, eng=DVE):
        return S.add(eng, lambda e: e.tensor_tensor(out.ap, in0.ap, in1.ap, op), [in0, in1], [out])

    def ts(out, in0, s1, op0, s2=None, op1=None, eng=DVE):
        rd = [in0]
        a1 = s1.ap if isinstance(s1, V) else float(s1)
        if isinstance(s1, V):
            rd.append(s1)
        if op1 is None:
            return S.add(eng, lambda e: e.tensor_scalar(out.ap, in0.ap, a1, None, op0), rd, [out])
        a2 = s2.ap if isinstance(s2, V) else float(s2)
        if isinstance(s2, V):
            rd.append(s2)
        return S.add(eng, lambda e: e.tensor_scalar(out.ap, in0.ap, a1, a2, op0, op1), rd, [out])

    def stt(out, in0, scl, in1, op0, op1):
        rd = [in0, in1]
        a = scl.ap if isinstance(scl, V) else float(scl)
        if isinstance(scl, V):
            rd.append(scl)
        return S.add(DVE, lambda e: e.scalar_tensor_tensor(out.ap, in0.ap, a, in1.ap, op0, op1), rd, [out])

    def cp(out, in_, eng=DVE):
        return S.add(eng, lambda e: e.tensor_copy(out.ap, in_.ap), [in_], [out])

    def recip(out, in_):
        return S.add(DVE, lambda e: e.reciprocal(out.ap, in_.ap), [in_], [out])

    def mset(out, val, eng=POOL):
        return S.add(eng, lambda e: e.memset(out.ap, val), [], [out])

    def dma(out, in_, eng=SP):
        return S.add(eng, lambda e: e.dma_start(out=out.ap, in_=in_.ap), [in_], [out], dma=True)

    def cv(o, n):
        return CST[o:o + n]

    def chk(name, tiles):
        if debug != name:
            return
        o = 0
        for tl, n in tiles:
            dma(dbg.v(o, [(4096, 128), (1, n)]), tl.a.v(tl.off, [(1, n)]), eng=POOL)
            o += n
        raise _Stop()

    dma(CST.all(), cst.v(0, [(CST_N, 128), (1, CST_N)]))
    dma(VEC.all(), vecs.v(0, [(VEC_N, 128), (1, VEC_N)]))
    dma(fing.all(), rows.v(R_FING, [(0, 128), (1, 1024)]))
    cp(ident.all(), cv(c_ident, 128), eng=POOL)
    mset(ones.all(), 1.0)
    pos_v = V(pos_h, 0, [(1, 32)], 4, pdim=(32, 0, 128))
    dma(pos_v, posT.v(0, [(32, 128), (1, 32)]))
    cp(posf.all(), pos_v)
    for s in range(32):
        ts(ang[s], cv(c_invf, 64), posf[s:s + 1], ALU.mult)
    ts(kk.all(), ang.all(), 1.0 / TWO_PI, ALU.mult, MAGIC, ALU.add)
    ts(kk.all(), kk.all(), -MAGIC, ALU.add)
    stt(ang.all(), kk.all(), -C1, ang.all(), ALU.mult, ALU.add)
    stt(ang.all(), kk.all(), -C2, ang.all(), ALU.mult, ALU.add)
    ts(kk.all(), ang.all(), math.pi / 2, ALU.is_gt, -TWO_PI, ALU.mult)
    stt(kk.all(), ang.all(), math.pi / 2, kk.all(), ALU.add, ALU.add)
    ts(ang.all(), ang.all(), -3.141592, ALU.max, 3.141592, ALU.min)
    ts(kk.all(), kk.all(), -3.141592, ALU.max, 3.141592, ALU.min)
    act(sinT.all(), ang.all(), AF.Sin)
    act(cosT.all(), kk.all(), AF.Sin)

    def w2d(dr, l, K, C, r0, c0, nk, ncol):
        return dr.v(l * K * C + r0 * C + c0, [(C, 128), (128 * C, nk), (1, ncol)])

    def blocks_for(l):
        B = {}

        def colblk(dr, K, C, c0):
            return [(None, w2d(dr, l, K, C, 0, c0, 8, 512))]
        B[0] = colblk(w_in, D, IN_COLS, OFF_Q)
        B[1] = colblk(w_in, D, IN_COLS, OFF_K)
        B[2] = colblk(w_in, D, IN_COLS, OFF_V)
        B[3] = colblk(w_in, D, IN_COLS, OFF_V + 512)
        B[4] = colblk(w_in, D, IN_COLS, OFF_G)
        B[5] = colblk(w_in, D, IN_COLS, OFF_G + 512)
        B[6] = colblk(w_in, D, IN_COLS, OFF_POOL)
        B[7] = colblk(w_in, D, IN_COLS, OFF_XQ)
        for hf in range(2):
            for b in range(3):
                B[8 + 5 * hf + b] = colblk(w_in, D, IN_COLS, OFF_GATE + b * 1024 + hf * 512)
            B[11 + 5 * hf] = [((0, 4), w2d(w_up_pool, l, 512, D, 0, hf * 512, 4, 512)),
                              ((4, 4), w2d(w_up_mem, l, 512, D, 0, hf * 512, 4, 512))]
            B[12 + 5 * hf] = colblk(w_up_ret, 1024, D, hf * 512)
            B[18 + hf] = colblk(w_out, D, D, hf * 512)
        for b in range(8):
            B[20 + b] = colblk(w_mlp1, D, 4096, b * 512)
        for hf in range(2):
            for fb in range(4):
                B[28 + hf * 4 + fb] = [(None, w2d(w_mlp2, l, 4096, D, fb * 1024, hf * 512, 8, 512))]
        B[36] = colblk(w_mem_kv, D, 1024, 0)
        B[37] = colblk(w_mem_kv, D, 1024, 512)
        return B

    def wq_blk(l, b):
        return wq.v((l * NBLK + b) * 128 * 4096, [(4096, 128), (512, 8), (1, 512)])

    def cast_blocks(l, ids):
        B = blocks_for(l)
        for b in ids:
            cast_done.add((l, b))
            for part, src in B[b]:
                base = (l * NBLK + b) * 128 * 4096
                if part is None:
                    dst = wq.v(base, [(4096, 128), (512, 8), (1, 512)])
                else:
                    dst = wq.v(base + part[0] * 512, [(4096, 128), (512, part[1]), (1, 512)])
                dma(dst, src, eng=POOL)

    PRE_IDS = [1, 2, 3, 6]
    ALL_IDS = [36, 37] + list(range(36))
    REST_IDS = [b for b in ALL_IDS if b not in PRE_IDS]
    castq = []
    cast_done = set()

    def pump(n):
        for _ in range(min(n, len(castq))):
            l, b = castq.pop(0)
            cast_blocks(l, [b])

    state = {"ring": 0, "ps": 0}

    def ring_load(l, b):
        assert (l, b) in cast_done, (l, b)
        slot = ring[state["ring"] % RING]
        state["ring"] += 1
        dma(A16.v(slot.off, [(512, 8), (1, 512)]), wq_blk(l, b))
        return slot

    def ps_next():
        a = PS[state["ps"] % 2]
        state["ps"] += 1
        return a

    gvec = lambda base, l: Tl(A32, VEC.off + base + l * 8, [8])
    pscale = lambda l: Tl(A32, VEC.off + V_PSC + l * 4, [4])

    def rms_rstd(src_rows, n, dim):
        for i, v in enumerate(src_rows):
            act(junk[0:dim], v, AF.Square, accum=ss[i:i + 1])
        ts(rstd[0:n], ss[0:n], 1.0 / dim, ALU.mult, EPS, ALU.add, eng=POOL)
        tt(rstd[0:n], rstd[0:n], cv(c_neghalf, n), ALU.pow, eng=POOL)

    def norm_to_hT(gv):
        rms_rstd([X[st] for st in range(NST)], NST, D)
        for st in range(NST):
            ts(hn[st], X[st], rstd[st:st + 1], ALU.mult)
        for kc in range(8):
            pb = PB[kc % 2]
            for st in range(NST):
                tp(pb.v(st * 128, [(1, 128)]), hn[st, kc * 128:(kc + 1) * 128])
            if kc % 2:
                ts(hT[kc], pb.v(0, [(1, 512)]), gv[kc:kc + 1], ALU.mult)
            else:
                act(hT[kc], pb.v(0, [(1, 512)]), AF.Copy, scale=gv[kc:kc + 1])

    def load_x(src, t, buf):
        for st in range(NST):
            dma(buf[st], src.v((t * T + st * 128) * D, [(D, 128), (1, D)]))

    def proj_tok(blk, st, evac):
        ps = ps_next()
        for kc in range(8):
            mm(ps.v(0, [(1, 512)]), hT[kc, st * 128:(st + 1) * 128], blk[kc], start=(kc == 0), stop=(kc == 7))
        evac(ps)

    def proj_feat(blk, j, evac, nk=8, rhs=None, kofs=0):
        ps = ps_next()
        rhs = rhs or hT
        for kc in range(nk):
            mm(ps.v(0, [(1, 512)]), blk[kofs + kc, j * 128:(j + 1) * 128], rhs[kc], start=(kc == 0), stop=(kc == nk - 1))
        evac(ps)

    def rotary(ps, gst, dst, kscale):
        src4 = ps.v(0, [(64, 8), (1, 64)])
        cosb = A32.v(cosT.off + gst * 64, [(0, 8), (1, 64)])
        sinb = A32.v(sinT.off + gst * 64, [(0, 8), (1, 64)])
        tt(A32.v(rotA.off, [(64, 8), (1, 64)]), src4, cosb, ALU.mult)
        tt(A32.v(rotB.off, [(64, 8), (1, 64)]), src4, sinb, ALU.mult)
        a1 = A32.v(rotA.off, [(128, 4), (1, 64)])
        a2 = A32.v(rotA.off + 64, [(128, 4), (1, 64)])
        b1 = A32.v(rotB.off, [(128, 4), (1, 64)])
        b2 = A32.v(rotB.off + 64, [(128, 4), (1, 64)])
        if kscale:
            o1 = A32.v(rotR.off, [(128, 4), (1, 64)])
            o2 = A32.v(rotR.off + 64, [(128, 4), (1, 64)])
        else:
            o1 = A16.v(dst.off, [(128, 4), (1, 64)])
            o2 = A16.v(dst.off + 64, [(128, 4), (1, 64)])
        tt(o1, a1, b2, ALU.subtract, eng=POOL)
        tt(o2, a2, b1, ALU.add, eng=POOL)
        if kscale:
            tt(dst.all(), rotR.all(), cv(c_kscale, 512), ALU.mult, eng=POOL)

    def sub(tl, i):
        return Tl(tl.a, tl.off + i * tl.strides[0], tl.shape[1:])

    def kv_update(c, chunk_idx):
        for hp in range(2):
            ps = PS[3]
            for hh in range(2):
                h = hp * 2 + hh
                mm(ps.v(hh * 256, [(1, 256)]), kpr[c, h * 128:(h + 1) * 128], vv[c, h * 256:(h + 1) * 256])
            for hh in range(2):
                h = hp * 2 + hh
                stt(Z[h], Z[h], GC[h], ps.v(hh * 256, [(1, 256)]), ALU.mult, ALU.add)

    def snap_Sb(c):
        for h in range(H):
            act(Sbs[c][h], Z[h], AF.Copy, scale=GC[h])

    def exchange_out(l, last_U):
        cp(exl[0:1024], Z.all(), eng=POOL)
        cp(A32.v(exl.off + 1024, [(16, 4), (1, 16)]), A32.v(U.off + 512, [(528, 4), (1, 16)]), eng=POOL)
        dma(exs[l].v(0, [(EXN, 128), (1, EXN)]), exl.all())

    def exchange_in(l):
        if fused:
            S.add(POOL, lambda e: e.collective_compute("AllGather", ALU.bypass, [[0, 1], [2, 3], [4, 5], [6, 7]],
                                                       ins=[exs[l].v(0, [(EXN, 128), (1, EXN)]).ap],
                                                       outs=[exg[l].v(0, [(EXN, 256), (1, EXN)]).ap]),
                  [exs[l].v(0, [(EXN, 128), (1, EXN)])], [exg[l].v(0, [(EXN, 256), (1, EXN)])], cc=True)
        dma(exl.all(), exg[l].v(0, [(EXN, 128), (1, EXN)]))
        ts(Z.all(), exl[0:1024], cv(c_isB, 1), ALU.mult)
        ts(halo.all(), exl[1024:1088], cv(c_isB, 1), ALU.mult)

    def mem_pre(l):
        for c in range(2):
            dma(Xb[0][c], mem_in.v(c * 128 * D, [(D, 128), (1, D)]))
        rms_rstd([Xb[0][c] for c in range(2)], 2, D)
        for c in range(2):
            ts(memn[c], Xb[0][c], rstd[c:c + 1], ALU.mult)
        gv = gvec(V_GMEM, l)
        for kc in range(8):
            pb = PB[kc % 2]
            for c in range(2):
                tp(pb.v(c * 128, [(1, 128)]), memn[c, kc * 128:(kc + 1) * 128])
            ts(memnT[kc], pb.v(0, [(1, 256)]), gv[kc:kc + 1], ALU.mult)
        bk = ring_load(l, 36)
        for h in range(H):
            ps = ps_next()
            for kc in range(8):
                mm(ps.v(0, [(1, 256)]), bk[kc, h * 128:(h + 1) * 128], memnT[kc], start=(kc == 0), stop=(kc == 7))
            act(kmT[h], ps.v(0, [(1, 256)]), AF.Copy)
        bv = ring_load(l, 37)
        for c in range(2):
            ps = ps_next()
            for kc in range(8):
                mm(ps.v(0, [(1, 512)]), memnT[kc, c * 128:(c + 1) * 128], bv[kc], start=(kc == 0), stop=(kc == 7))
            act(vm[c], ps.v(0, [(1, 512)]), AF.Copy)

    def prepass(l, src):
        mset(Z.all(), 0.0)
        gv = gvec(V_GMIX, l)
        nxt = None
        for t in range(NT):
            pump(5)
            if t == 0:
                load_x(src, t, X.flip())
            else:
                X.flip()
            norm_to_hT(gv)
            if t + 1 < NT:
                load_x(src, t + 1, Xb[(X.n + 1) % 2])
            bk = ring_load(l, 1)
            for st in range(NST):
                proj_tok(bk, st, lambda ps, st=st: rotary(ps, t * NST + st, sub(kpr, st), True))
            for hf in range(2):
                bv = ring_load(l, 2 + hf)
                for st in range(NST):
                    proj_tok(bv, st, lambda ps, st=st, hf=hf: act(vv[st, hf * 512:(hf + 1) * 512], ps.v(0, [(1, 512)]), AF.Copy))
            for c in range(NST):
                kv_update(c, t * NST + c)
            if t == NT - 1:
                bp = ring_load(l, 6)
                for g in range(4):
                    proj_feat(bp, g, lambda ps, g=g: act(U[g, 16:528], ps.v(0, [(1, 512)]), AF.Copy))
        exchange_out(l, None)

    def main(l, src, dst_x, final):
        gv = gvec(V_GMIX, l)
        gv2 = gvec(V_GMLP, l)
        dma(retg.all(), rows.v(R_RETG + l * 1024, [(0, 128), (1, 1024)]))
        dma(A16.v(poolw.off, [(128, 4), (1, 128)]), pool_w.v(l * 4 * 128 * 128, [(128, 128), (128 * 128, 4), (1, 128)]), eng=POOL)
        for t in range(NT):
            pump(5)
            if t == 0:
                load_x(src, t, X.flip())
            else:
                X.flip()
            norm_to_hT(gv)
            if t + 1 < NT:
                load_x(src, t + 1, Xb[(X.n + 1) % 2])
            bq = ring_load(l, 0)
            for st in range(NST):
                proj_tok(bq, st, lambda ps, st=st: rotary(ps, t * NST + st, sub(qrot, st), False))
            bk = ring_load(l, 1)
            for st in range(NST):
                proj_tok(bk, st, lambda ps, st=st: rotary(ps, t * NST + st, sub(kpr, st), True))
            for hf in range(2):
                bv = ring_load(l, 2 + hf)
                for st in range(NST):
                    proj_tok(bv, st, lambda ps, st=st, hf=hf: act(vv[st, hf * 512:(hf + 1) * 512], ps.v(0, [(1, 512)]), AF.Copy))
            for hf in range(2):
                bg = ring_load(l, 4 + hf)
                for st in range(NST):
                    def ev(ps, st=st, hf=hf):
                        act(gs[st, hf * 512:(hf + 1) * 512], ps.v(0, [(1, 512)]), AF.Silu)
                        tt(gs[st, hf * 512:(hf + 1) * 512], gs[st, hf * 512:(hf + 1) * 512],
                           retg[hf * 512:(hf + 1) * 512], ALU.mult, eng=POOL)
                    proj_tok(bg, st, ev)
            chk("proj", [(qrot, 512), (kpr, 512), (vv, 1024), (gs, 1024)])
            for c in range(NST):
                snap_Sb(c)
                kv_update(c, t * NST + c)
            ysb = Tl(A32, rotA.off, [4, 256])

            def stageA(c):
                par = c % 2
                for h in range(H):
                    tp(PB[0].v(h * 128, [(1, 128)]), qrot[c, h * 128:(h + 1) * 128])
                    tp(PB[1].v(h * 128, [(1, 128)]), kpr[c, h * 128:(h + 1) * 128])
                act(qT[par].all(), PB[0].v(0, [(1, 512)]), AF.Copy)
                cp(kT[par].all(), PB[1].v(0, [(1, 512)]))
                for h in range(H):
                    mm(PS[2].v(h * 128, [(1, 128)]), kT[par][h], qT[par][h])
                tt(scm[par].all(), PS[2].v(0, [(1, 512)]), cv(c_cmask, 512), ALU.mult)

            def stageB(c):
                par = c % 2
                for h in range(H):
                    ps = ps_next()
                    mm(ps.v(0, [(1, 256)]), scm[par][h], vv[c, h * 256:(h + 1) * 256])
                    mm(ps.v(256, [(1, 256)]), qT[par][h], Sbs[c][h])
                    act(ysb[h], ps.v(0, [(1, 256)]), AF.Copy)
                    stt(ysb[h], ysb[h], 1.0, ps.v(256, [(1, 256)]), ALU.mult, ALU.add)
                for h in range(H):
                    act(junk[0:256], ysb[h], AF.Square, accum=ys[h:h + 1])
                tt(ys[4:8], ys[0:4], cv(c_yscale2, 4), ALU.mult, eng=POOL)
                ts(ys[4:8], ys[4:8], 1.0, ALU.mult, EPS, ALU.add, eng=POOL)
                tt(ys[4:8], ys[4:8], cv(c_neghalf, 4), ALU.pow, eng=POOL)
                tt(sc[0:4], ys[4:8], cv(c_yscale, 4), ALU.mult, eng=POOL)
                for h in range(H):
                    ts(ysb[h], ysb[h], sc[h:h + 1], ALU.mult)
                    tt(yret[par][h * 256:(h + 1) * 256], ysb[h], gs[c, h * 256:(h + 1) * 256], ALU.mult, eng=POOL)
                pb = PB[2 + par]
                for kc in range(8):
                    tp(pb.v(kc * 128, [(1, 128)]), yret[par][kc * 128:(kc + 1) * 128])
                act(A16.v(yretT.off + c * 128, [(512, 8), (1, 128)]), pb.v(0, [(128, 8), (1, 128)]), AF.Copy)

            for c in range(NST):
                stageA(c)
                if c >= 1:
                    stageB(c - 1)
            stageB(NST - 1)
            chk("ret", [(yretT, 4096)])
            cp(A32.v(U.off, [(528, 4), (1, 16)]), A32.v(halo.off, [(16, 4), (1, 16)]), eng=POOL)
            bp = ring_load(l, 6)
            for g in range(4):
                proj_feat(bp, g, lambda ps, g=g: act(U[g, 16:528], ps.v(0, [(1, 512)]), AF.Copy))
            cp(A32.v(halo.off, [(16, 4), (1, 16)]), A32.v(U.off + 512, [(528, 4), (1, 16)]), eng=POOL)
            for g in range(4):
                w = 2 << g
                cur, curlen, shift = sub(U, g), 528, 1
                bufs = [tmpA, tmpB]
                bi = 0
                while shift < w:
                    nl = curlen - shift
                    nxt = bufs[bi]
                    tt(nxt[0:nl], cur[shift:curlen], cur[0:nl], ALU.add, eng=POOL)
                    cur, curlen = nxt, nl
                    bi ^= 1
                    shift *= 2
                o = 17 - w
                stt(dd[g], cur[o:o + 512], 1.0 / w, U[g, 16:528], ALU.mult, ALU.subtract)
                if t == 0:
                    tt(mtmp[0, 0:16], cur[o:o + 16], cv(c_invcnt + g * 16, 16), ALU.mult)
                    tt(dd[g, 0:16], mtmp[0, 0:16], U[g, 16:32], ALU.subtract)
            psc = pscale(l)
            for g in range(4):
                ps = ps_next()
                mm(ps.v(0, [(1, 512)]), poolw[g], dd[g])
                act(ypoolT[g], ps.v(0, [(1, 512)]), AF.Copy, scale=psc[g:g + 1])
            chk("pool", [(ypoolT, 2048)])
            bx = ring_load(l, 7)
            for h in range(H):
                proj_feat(bx, h, lambda ps, h=h: act(xqT[h], ps.v(0, [(1, 512)]), AF.Copy))
            for h in range(H):
                par = h % 2
                for c in range(2):
                    ps = ps_next()
                    mm(ps.v(0, [(1, 512)]), kmT[h, c * 128:(c + 1) * 128], xqT[h])
                    act(pT[par][c], ps.v(0, [(1, 512)]), AF.Exp, scale=128 ** -0.5)
                pso = ps_next()
                for c in range(2):
                    mm(pso.v(0, [(1, 512)]), vm[c, h * 128:(h + 1) * 128], pT[par][c], start=(c == 0), stop=(c == 1))
                for c in range(2):
                    mm(PS[2].v(0, [(1, 512)]), ones.all(), pT[par][c], start=(c == 0), stop=(c == 1))
                recip(rsum.all(), PS[2].v(0, [(1, 512)]))
                tt(ymemT[h], pso.v(0, [(1, 512)]), rsum.all(), ALU.mult)
            chk("mem", [(ymemT, 2048)])
            for hf in range(2):
                for b in range(3):
                    bgate = ring_load(l, 8 + 5 * hf + b)
                    for j in range(4):
                        proj_feat(bgate, j, lambda ps, b=b, j=j: act(sg[b, j], ps.v(0, [(1, 512)]), AF.Sigmoid))
                bpm = ring_load(l, 11 + 5 * hf)
                bret = ring_load(l, 12 + 5 * hf)
                for j in range(4):
                    proj_feat(bpm, j, lambda ps, j=j: tt(mtmp[0], ps.v(0, [(1, 512)]), sg[0, j], ALU.mult), nk=4, rhs=ypoolT)
                    proj_feat(bret, j, lambda ps, j=j: tt(mtmp[1], ps.v(0, [(1, 512)]), sg[1, j], ALU.mult), nk=8, rhs=yretT)
                    tt(mtmp[0], mtmp[0], mtmp[1], ALU.add, eng=POOL)
                    proj_feat(bpm, j, lambda ps, j=j: tt(mtmp[1], ps.v(0, [(1, 512)]), sg[2, j], ALU.mult), nk=4, rhs=ymemT, kofs=4)
                    tt(mergedT[hf * 4 + j], mtmp[0], mtmp[1], ALU.add, eng=POOL)
            chk("merge", [(mergedT, 4096)])
            for hf in range(2):
                bo = ring_load(l, 18 + hf)
                for st in range(NST):
                    ps = ps_next()
                    for kc in range(8):
                        mm(ps.v(0, [(1, 512)]), mergedT[kc, st * 128:(st + 1) * 128], bo[kc], start=(kc == 0), stop=(kc == 7))
                    tt(X[st, hf * 512:(hf + 1) * 512], ps.v(0, [(1, 512)]), X[st, hf * 512:(hf + 1) * 512], ALU.add)
            chk("out", [(X.cur, 4096)])
            norm_to_hT(gv2)
            for b in range(8):
                b1 = ring_load(l, 20 + b)
                for j in range(4):
                    def ev(ps, b=b, j=j):
                        act(mtmp[j % 2], ps.v(0, [(1, 512)]), AF.Relu)
                        tt(hid[b * 4 + j], mtmp[j % 2], mtmp[j % 2], ALU.mult, eng=POOL)
                    proj_feat(b1, j, ev)
            for hf in range(2):
                b2s = [ring_load(l, 28 + hf * 4 + fb) for fb in range(4)]
                for st in range(NST):
                    ps = ps_next()
                    for fb in range(4):
                        for k8 in range(8):
                            mm(ps.v(0, [(1, 512)]), hid[fb * 8 + k8, st * 128:(st + 1) * 128], b2s[fb][k8],
                               start=(fb == 0 and k8 == 0), stop=(fb == 3 and k8 == 7))
                    tt(X[st, hf * 512:(hf + 1) * 512], ps.v(0, [(1, 512)]), X[st, hf * 512:(hf + 1) * 512], ALU.add)
            chk("mlp", [(X.cur, 4096)])
            if final:
                rms_rstd([X[st] for st in range(NST)], NST, D)
                for st in range(NST):
                    stt(X[st], X[st], rstd[st:st + 1], fing.all(), ALU.mult, ALU.mult)
            for st in range(NST):
                dma(dst_x.v((t * T + st * 128) * D, [(D, 128), (1, D)]), X[st])

    def _program():
        if do_pre0 and not fused:
            cast_blocks(0, PRE_IDS)
            prepass(0, x_in)
        if fused:
            cast_blocks(0, PRE_IDS)
            castq.extend([(0, b) for b in REST_IDS] + [(1, b) for b in PRE_IDS] + [(1, b) for b in REST_IDS])
            prepass(0, x_in)
            pump(len(REST_IDS) + len(PRE_IDS) - 40 if len(REST_IDS) + len(PRE_IDS) > 40 else 0)
            while castq and castq[0][0] == 0:
                pump(1)
            exchange_in(0)
            mem_pre(0)
            main(0, x_in, xs, False)
            while castq and castq[0] in [(1, b) for b in PRE_IDS]:
                pump(1)
            prepass(1, xs)
            pump(len(castq))
            exchange_in(1)
            mem_pre(1)
            main(1, xs, out_d, True)
            return
        if do_main0:
            cast_blocks(0, ALL_IDS)
            cast_blocks(1, PRE_IDS)
            exchange_in(0)
            mem_pre(0)
            main(0, x_in, xs, False)
            prepass(1, xs)
        if do_main1:
            cast_blocks(1, ALL_IDS)
            exchange_in(1)
            mem_pre(1)
            main(1, xs, out_d, True)

    try:
        _program()
    except _Stop:
        pass
    S.emit(nc)
    return nc


def _consts(core):
    half = core % 2
    c = np.zeros((128, CST_N), np.float32)
    j = np.arange(128)
    c[:, c_ident:c_ident + 128] = np.eye(128, dtype=np.float32)
    cm = (j[None, :] >= j[:, None]).astype(np.float32)
    c[:, c_cmask:c_cmask + 512] = np.tile(cm, (1, 4))
    for h in range(H):
        g = np.float64(GAMMA[h])
        c[:, c_kscale + h * 128:c_kscale + (h + 1) * 128] = (g ** (-(j + 1.0)) * 128 ** -0.5)[:, None]
        c[:, c_yscale + h] = g ** (j + 1.0)
        c[:, c_yscale2 + h] = g ** (2.0 * (j + 1.0)) / 256.0
        c[:, c_gcb + h] = g ** 128 * half
    c[:, c_invf:c_invf + 64] = (10000.0 ** (-np.arange(64, dtype=np.float64) / 64.0)).astype(np.float32)[None, :]
    for gi, w in enumerate((2, 4, 8, 16)):
        tt_ = np.arange(16)
        cnt = np.minimum(tt_ + 1, w) if half == 0 else np.full(16, w)
        c[:, c_invcnt + gi * 16:c_invcnt + (gi + 1) * 16] = (1.0 / cnt)[None, :]
    c[:, c_isB] = float(half)
    c[:, c_neghalf:c_neghalf + 8] = -0.5
    return c


def _in_map(core, inp):
    b, half = core // 2, core % 2
    sl = slice(half * TOK, (half + 1) * TOK)
    f = lambda a: np.ascontiguousarray(a, dtype=np.float32)
    vec = np.zeros((128, VEC_N), np.float32)
    for l in range(2):
        vec[:, V_GMIX + l * 8:V_GMIX + l * 8 + 8] = inp["norm_mix_g"][l].reshape(8, 128).T
        vec[:, V_GMLP + l * 8:V_GMLP + l * 8 + 8] = inp["norm_mlp_g"][l].reshape(8, 128).T
        vec[:, V_GMEM + l * 8:V_GMEM + l * 8 + 8] = inp["mem_norm_g"][l].reshape(8, 128).T
        vec[:, V_PSC + l * 4:V_PSC + l * 4 + 4] = inp["pool_scale"][l].reshape(4, 128).T
    rows = np.concatenate([inp["ret_norm_g"][0], inp["ret_norm_g"][1], inp["final_norm_g"]]).astype(np.float32)
    return {
        "x": f(inp["x"][b, sl]),
        "mem": f(inp["mem"][b]),
        "posT": np.ascontiguousarray(np.asarray(inp["positions"])[b, sl].reshape(32, 128).T.astype(np.int32)),
        "cst": _consts(core),
        "vecs": vec,
        "rows": rows,
        "w_in": f(inp["w_in"]), "pool_w": f(inp["pool_w"]), "w_mem_kv": f(inp["w_mem_kv"]),
        "w_up_pool": f(inp["w_up_pool"]), "w_up_ret": f(inp["w_up_ret"]), "w_up_mem": f(inp["w_up_mem"]),
        "w_out": f(inp["w_out"]), "w_mlp1": f(inp["w_mlp1"]), "w_mlp2": f(inp["w_mlp2"]),
    }


FUSED = True


def kernel(**inputs):
    inp = {k: np.asarray(v) for k, v in inputs.items()}
    maps = [_in_map(c, inp) for c in range(NCORES)]
    cores = list(range(NCORES))
    if FUSED:
        nc = build("fused")
        res = run_bass_kernel_spmd(nc, maps, core_ids=cores)
        outs = [r["out"] for r in res.results]
    else:
        def partner(arrs):
            return [arrs[c - 1] if c % 2 else arrs[c] for c in range(NCORES)]
        r0 = run_bass_kernel_spmd(build("unfused", 0), maps, core_ids=cores).results
        ex0 = partner([r["exs0"] for r in r0])
        m1 = [dict(m, exg0=ex0[c]) for c, m in enumerate(maps)]
        r1 = run_bass_kernel_spmd(build("unfused", 1), m1, core_ids=cores).results
        ex1 = partner([r["exs1"] for r in r1])
        m2 = [dict(m, exg1=ex1[c], xs=r1[c]["xs"]) for c, m in enumerate(maps)]
        r2 = run_bass_kernel_spmd(build("unfused", 2), m2, core_ids=cores).results
        outs = [r["out"] for r in r2]
    out = np.empty((4, 8192, D), np.float32)
    for c in range(NCORES):
        out[c // 2, (c % 2) * TOK:(c % 2 + 1) * TOK] = outs[c]
    return out
```

```python
import math
import numpy as np
import concourse.bass as bass
import concourse.mybir as mybir
from concourse.bass_utils import run_bass_kernel_spmd

F32 = mybir.dt.float32
BF16 = mybir.dt.bfloat16
I32 = mybir.dt.int32
AF = mybir.ActivationFunctionType
ALU = mybir.AluOpType

NCORES = 8
D = 1024
TOK = 4096
T = 512
NT = TOK // T
NST = 4
H = 4
IN_COLS = 7168
OFF_POOL, OFF_Q, OFF_K, OFF_V, OFF_G, OFF_XQ, OFF_GATE = 0, 512, 1024, 1536, 2560, 3584, 4096
EPS = 1e-6
GAMMA = [1.0 - 2.0 ** (-5.0 - h) for h in range(H)]
GC = [g ** 128 for g in GAMMA]
NBLK = 38
RING = 4
MAGIC = 12582912.0
TWO_PI = 2.0 * math.pi
C1 = 6.28125
C2 = TWO_PI - C1
EXN = 1024 + 64

c_ident = 0
c_cmask = c_ident + 128
c_kscale = c_cmask + 512
c_yscale = c_kscale + 512
c_yscale2 = c_yscale + 4
c_invf = c_yscale2 + 4
c_invcnt = c_invf + 64
c_isB = c_invcnt + 64
c_neghalf = c_isB + 1
c_gcb = c_neghalf + 8
CST_N = c_gcb + 4
V_GMIX, V_GMLP, V_GMEM, V_PSC = 0, 16, 32, 48
VEC_N = 56
R_RETG, R_FING = 0, 2048
ROW_N = 3072


class V:
    __slots__ = ("ap", "key", "lo", "hi")

    def __init__(self, handle, off, dims, esz, pdim=None, track=True):
        if pdim is not None:
            F, p0, np_ = pdim
            self.ap = bass.AP(handle, p0 * F + off, [[F, np_]] + [[s, n] for s, n in dims])
        else:
            self.ap = bass.AP(handle, off, [[s, n] for s, n in dims])
        lo = off + sum(min(0, s * (n - 1)) for s, n in dims)
        hi = off + sum(max(0, s * (n - 1)) for s, n in dims) + 1
        self.key = handle.name if track else None
        self.lo, self.hi = lo * esz, hi * esz


class Arena:
    def __init__(self, nc, name, nelem, dtype, esz, psum=False):
        self.F = nelem
        self.esz = esz
        self.h = (nc.alloc_psum_tensor if psum else nc.alloc_sbuf_tensor)(name, [128, nelem], dtype)
        self.ptr = 0

    def alloc(self, n):
        o = self.ptr
        self.ptr += n
        assert self.ptr <= self.F, (self.h.name, self.ptr, self.F)
        return o

    def v(self, off, dims, p0=0, np_=128):
        return V(self.h, off, dims, self.esz, pdim=(self.F, p0, np_))


class Tl:
    def __init__(self, arena, off, shape):
        self.a, self.off, self.shape = arena, off, tuple(shape)
        st = []
        acc = 1
        for n in reversed(self.shape):
            st.append(acc)
            acc *= n
        self.strides = tuple(reversed(st))
        self.size = acc

    def __getitem__(self, idx):
        if not isinstance(idx, tuple):
            idx = (idx,)
        off = self.off
        dims = []
        for i, n in enumerate(self.shape):
            s = self.strides[i]
            ix = idx[i] if i < len(idx) else slice(None)
            if isinstance(ix, int):
                off += ix * s
            else:
                a = ix.start or 0
                b = n if ix.stop is None else ix.stop
                off += a * s
                dims.append((s, b - a))
        merged = []
        for s, n in dims:
            if merged and merged[-1][0] == s * n:
                merged[-1] = (s, merged[-1][1] * n)
            else:
                merged.append((s, n))
        if not merged:
            merged = [(1, 1)]
        return self.a.v(off, merged)

    def all(self):
        return self[tuple(slice(None) for _ in self.shape)]


class Op:
    __slots__ = ("eng", "fn", "deps", "dma", "sig", "needed")

    def __init__(self, eng, fn, dma):
        self.eng, self.fn, self.dma = eng, fn, dma
        self.deps = set()
        self.sig = None
        self.needed = False


class Sched:
    ENGS = ("pe", "act", "dve", "pool", "sp")

    def __init__(self, ndma=8):
        self.ops = {e: [] for e in self.ENGS}
        self.track = {}
        self.ndma = ndma
        self.dma_ops = {e: [] for e in self.ENGS}

    def _access(self, v, op, is_write):
        if v.key is None:
            return
        segs = self.track.get(v.key, [])
        lo, hi = v.lo, v.hi
        if is_write and op.eng == "pe" and v.key[:2] in ("ps", "pb"):
            lo, hi = 0, 2048
        out = []
        cover = []
        for sg in segs:
            slo, shi, w, rs = sg
            if shi <= lo or slo >= hi:
                out.append(sg)
                continue
            if slo < lo:
                out.append([slo, lo, w, list(rs)])
            if shi > hi:
                out.append([hi, shi, w, list(rs)])
            cover.append([max(slo, lo), min(shi, hi), w, rs])
        for c in cover:
            if c[2] is not None:
                self._dep(op, c[2], raw=not is_write)
            if is_write:
                for r in c[3]:
                    self._dep(op, r, raw=False)
        if is_write:
            out.append([lo, hi, op, []])
        else:
            cover.sort(key=lambda c: c[0])
            pos = lo
            for c in cover:
                if c[0] > pos:
                    out.append([pos, c[0], None, [op]])
                out.append([c[0], c[1], c[2], list(c[3]) + [op]])
                pos = c[1]
            if pos < hi:
                out.append([pos, hi, None, [op]])
        self.track[v.key] = out

    def _dep(self, op, other, raw):
        if other is op:
            return
        if other.eng == op.eng == "pe" and not other.dma and not op.dma:
            return
        op.deps.add(other)

    def add(self, eng, fn, reads=(), writes=(), dma=False, cc=False):
        op = Op(eng, fn, dma or cc)
        self.cc_ops = getattr(self, "cc_ops", [])
        for v in reads:
            self._access(v, op, False)
        for v in writes:
            self._access(v, op, True)
        if cc:
            self.cc_ops.append(op)
        if dma:
            lst = self.dma_ops[eng]
            if len(lst) >= self.ndma:
                op.deps.add(lst[len(lst) - self.ndma])
            lst.append(op)
        self.ops[eng].append(op)
        return op

    def emit(self, nc):
        for e in self.ENGS:
            for op in self.ops[e]:
                for d in op.deps:
                    d.needed = True
        import contextlib
        with contextlib.ExitStack() as es:
            esem = {e: es.enter_context(nc.semaphore("s_" + e)) for e in self.ENGS}
            dsem = {e: [es.enter_context(nc.semaphore("d_%s%d" % (e, i))) for i in range(self.ndma)]
                    for e in self.ENGS if self.dma_ops[e]}
            ccs = getattr(self, "cc_ops", [])
            ccsem = es.enter_context(nc.semaphore("s_cc")) if ccs else None
            for i, op in enumerate(ccs):
                op.sig = (ccsem, i + 1, 1)
            for e in self.ENGS:
                cnt = 0
                dcnt = 0
                for op in self.ops[e]:
                    if op.sig is not None:
                        continue
                    if op.dma:
                        k = dcnt % self.ndma
                        op.sig = (dsem[e][k], 16 * (dcnt // self.ndma + 1), 16)
                        dcnt += 1
                    elif op.needed:
                        cnt += 1
                        op.sig = (esem[e], cnt, 1)
            block = es.enter_context(nc.Block())
            engmap = {"pe": block.tensor, "act": block.scalar, "dve": block.vector, "pool": block.gpsimd,
                      "sp": block.sync}
            final_dma = [op for e in self.ENGS for op in self.dma_ops[e]]
            for e in self.ENGS:
                ops = self.ops[e]
                last = e == "sp"

                def body(eng, ops=ops, last=last):
                    waited = {}
                    for op in ops:
                        for d in sorted(op.deps, key=lambda d: d.sig[1]):
                            sem, val, _ = d.sig
                            if waited.get(id(sem), 0) < val:
                                eng.wait_ge(sem, val)
                                waited[id(sem)] = val
                        ins = op.fn(eng)
                        if op.sig is not None:
                            ins.then_inc(op.sig[0], op.sig[2])
                    if last:
                        for q in self.ENGS:
                            lst = self.dma_ops[q]
                            for op in lst[-self.ndma:]:
                                sem, val, _ = op.sig
                                if waited.get(id(sem), 0) < val:
                                    eng.wait_ge(sem, val)
                                    waited[id(sem)] = val

                engmap[e](body)


class DR:
    def __init__(self, nc, name, shape, dtype, kind, esz, track=True):
        self.h = nc.dram_tensor(name, list(shape), dtype, kind=kind)
        self.esz = esz
        self.track = track

    def v(self, off, dims):
        return V(self.h, off, dims, self.esz, pdim=None, track=self.track)


class _Stop(Exception):
    pass


def build(mode="fused", stage=0, debug=None):
    nc = bass.Bass("TRN2", target_bir_lowering=False)
    fused = mode == "fused"
    S = Sched()
    IN, OUT, INT = "ExternalInput", "ExternalOutput", "Internal"

    do_pre0 = fused or stage == 0
    do_main0 = fused or stage == 1
    do_pre1 = fused or stage == 1
    do_main1 = fused or stage == 2

    x_in = DR(nc, "x", [TOK, D], F32, IN, 4, track=False)
    mem_in = DR(nc, "mem", [256, D], F32, IN, 4, track=False)
    posT = DR(nc, "posT", [128, 32], I32, IN, 4, track=False)
    cst = DR(nc, "cst", [128, CST_N], F32, IN, 4, track=False)
    vecs = DR(nc, "vecs", [128, VEC_N], F32, IN, 4, track=False)
    rows = DR(nc, "rows", [ROW_N], F32, IN, 4, track=False)
    w_in = DR(nc, "w_in", [2, D, IN_COLS], F32, IN, 4, track=False)
    pool_w = DR(nc, "pool_w", [2, 4, 128, 128], F32, IN, 4, track=False)
    w_mem_kv = DR(nc, "w_mem_kv", [2, D, 1024], F32, IN, 4, track=False)
    w_up_pool = DR(nc, "w_up_pool", [2, 512, D], F32, IN, 4, track=False)
    w_up_ret = DR(nc, "w_up_ret", [2, 1024, D], F32, IN, 4, track=False)
    w_up_mem = DR(nc, "w_up_mem", [2, 512, D], F32, IN, 4, track=False)
    w_out = DR(nc, "w_out", [2, D, D], F32, IN, 4, track=False)
    w_mlp1 = DR(nc, "w_mlp1", [2, D, 4096], F32, IN, 4, track=False)
    w_mlp2 = DR(nc, "w_mlp2", [2, 4096, D], F32, IN, 4, track=False)
    wq = DR(nc, "wq", [2, NBLK, 128, 4096], BF16, INT, 2)
    if fused:
        out_d = DR(nc, "out", [TOK, D], F32, OUT, 4)
        xs = DR(nc, "xs", [TOK, D], F32, INT, 4)
        exs = [DR(nc, "exs%d" % l, [128, EXN], F32, INT, 4) for l in range(2)]
        exg = [DR(nc, "exg%d" % l, [256, EXN], F32, INT, 4) for l in range(2)]
    else:
        out_d = DR(nc, "out", [TOK, D], F32, OUT, 4) if stage == 2 else None
        xs = DR(nc, "xs", [TOK, D], F32, OUT if stage == 1 else IN, 4, track=(stage == 1)) if stage >= 1 else None
        exs = [None, None]
        exg = [None, None]
        if stage == 0:
            exs[0] = DR(nc, "exs0", [128, EXN], F32, OUT, 4)
        if stage == 1:
            exg[0] = DR(nc, "exg0", [128, EXN], F32, IN, 4, track=False)
            exs[1] = DR(nc, "exs1", [128, EXN], F32, OUT, 4)
        if stage == 2:
            exg[1] = DR(nc, "exg1", [128, EXN], F32, IN, 4, track=False)
    dbg = DR(nc, "dbg", [128, 4096], F32, OUT, 4) if debug else None

    A32 = Arena(nc, "a32", 21700, F32, 4)
    A16 = Arena(nc, "a16", 58200, BF16, 2)
    PS = [Arena(nc, "ps%d" % i, 512, F32, 4, psum=True) for i in range(4)]
    PB = [Arena(nc, "pb%d" % i, 1024, BF16, 2, psum=True) for i in range(4)]
    pos_h = nc.alloc_sbuf_tensor("pos_i", [128, 32], I32)

    def t32(shape):
        return Tl(A32, A32.alloc(int(np.prod(shape))), shape)

    def t16(shape):
        return Tl(A16, A16.alloc(int(np.prod(shape))), shape)

    cosT = t32([32, 64])
    sinT = t32([32, 64])
    CST = t32([CST_N])
    VEC = t32([VEC_N])
    retg = t32([1024])
    fing = t32([1024])
    Z = t32([H, 256])
    Xb = [t32([NST, 1024]), t32([NST, 1024])]

    class _XRef:
        cur = Xb[0]
        n = 0

        def __getitem__(self, idx):
            return self.cur[idx]

        def flip(self):
            _XRef.n += 1
            _XRef.cur = Xb[_XRef.n % 2]
            return _XRef.cur
    X = _XRef()
    ss = t32([8])
    rstd = t32([8])
    ys = t32([8])
    sc = t32([8])
    halo = t32([4, 16])
    tr32 = A32.ptr
    U = t32([4, 528])
    tmpA = t32([528])
    tmpB = t32([528])
    mtmp = t32([2, 512])
    rsum = t32([512])
    end_a = A32.ptr
    A32.ptr = tr32
    rotA = t32([512])
    rotB = t32([512])
    rotR = t32([512])
    posf = t32([32])
    A32.ptr = max(A32.ptr, end_a)
    exl = Tl(A32, tmpA.off, [EXN])
    ang = Tl(A32, Xb[0].off, [32, 64])
    kk = Tl(A32, Xb[0].off + 2048, [32, 64])
    ident = t16([128])
    ones = t16([128])
    Sbs = [t16([H, 256]) for _ in range(NST)]
    kmT = t16([H, 256])
    vm = t16([2, 512])
    poolw = t16([4, 128])
    ring = [t16([8, 512]) for _ in range(RING)]
    hT = t16([8, 512])
    hn = t16([NST, 1024])
    junk = t16([1024])
    tr16 = A16.ptr
    mergedT = t16([8, 512])
    yretT = t16([8, 512])
    ph = A16.ptr
    qrot = t16([NST, 512])
    kpr = t16([NST, 512])
    vv = t16([NST, 1024])
    gs = t16([NST, 1024])
    qT = [t16([H, 128]) for _ in range(2)]
    kT = [t16([H, 128]) for _ in range(2)]
    scm = [t16([H, 128]) for _ in range(2)]
    yret = [t16([1024]) for _ in range(2)]
    end_r = A16.ptr
    A16.ptr = ph
    dd = t16([4, 512])
    ypoolT = t16([4, 512])
    xqT = t16([4, 512])
    pT = [t16([2, 512]) for _ in range(2)]
    ymemT = t16([4, 512])
    sg = t16([3, 4, 512])
    end_p = A16.ptr
    A16.ptr = tr16 + 4096
    A16.ptr = tr16
    hid = t16([32, 512])
    memn = Tl(A16, hid.off, [2, 1024])
    memnT = Tl(A16, hid.off + 2048, [8, 256])
    A16.ptr = max(end_r, end_p, A16.ptr)

    PE, ACT, DVE, POOL, SP = "pe", "act", "dve", "pool", "sp"

    def mm(out, lhsT, rhs, start=True, stop=True):
        return S.add(PE, lambda e: e.matmul(out.ap, lhsT.ap, rhs.ap, start=start, stop=stop), [lhsT, rhs], [out])

    idv = ident.all()

    def tp(out, in_):
        return S.add(PE, lambda e: e.transpose(out.ap, in_.ap, idv.ap), [in_, idv], [out])

    def act(out, in_, func, scale=None, bias=None, accum=None):
        rd = [in_]
        kw = {}
        for nm, val in (("scale", scale), ("bias", bias)):
            if val is not None:
                if isinstance(val, V):
                    rd.append(val)
                    kw[nm] = val.ap
                else:
                    kw[nm] = float(val)
        wr = [out]
        if accum is not None:
            wr.append(accum)
            kw["accum_out"] = accum.ap
        return S.add(ACT, lambda e: e.activation(out.ap, in_.ap, func, **kw), rd, wr)

    def tt(out, in0, in1, op, eng=DVE):
        return S.add(eng, lambda e: e.tensor_tensor(out.ap, in0.ap, in1.ap, op), [in0, in1], [out])

    def ts(out, in0, s1, op0, s2=None, op1=None, eng=DVE):
        rd = [in0]
        a1 = s1.ap if isinstance(s1, V) else float(s1)
        if isinstance(s1, V):
            rd.append(s1)
        if op1 is None:
            return S.add(eng, lambda e: e.tensor_scalar(out.ap, in0.ap, a1, None, op0), rd, [out])
        a2 = s2.ap if isinstance(s2, V) else float(s2)
        if isinstance(s2, V):
            rd.append(s2)
        return S.add(eng, lambda e: e.tensor_scalar(out.ap, in0.ap, a1, a2, op0, op1), rd, [out])

    def stt(out, in0, scl, in1, op0, op1):
        rd = [in0, in1]
        a = scl.ap if isinstance(scl, V) else float(scl)
        if isinstance(scl, V):
            rd.append(scl)
        return S.add(DVE, lambda e: e.scalar_tensor_tensor(out.ap, in0.ap, a, in1.ap, op0, op1), rd, [out])

    def cp(out, in_, eng=DVE):
        return S.add(eng, lambda e: e.tensor_copy(out.ap, in_.ap), [in_], [out])

    def recip(out, in_):
        return S.add(DVE, lambda e: e.reciprocal(out.ap, in_.ap), [in_], [out])

    def mset(out, val, eng=POOL):
        return S.add(eng, lambda e: e.memset(out.ap, val), [], [out])

    def dma(out, in_, eng=SP):
        return S.add(eng, lambda e: e.dma_start(out=out.ap, in_=in_.ap), [in_], [out], dma=True)

    def cv(o, n):
        return CST[o:o + n]

    def chk(name, tiles):
        if debug != name:
            return
        o = 0
        for tl, n in tiles:
            dma(dbg.v(o, [(4096, 128), (1, n)]), tl.a.v(tl.off, [(1, n)]), eng=POOL)
            o += n
        raise _Stop()

    dma(CST.all(), cst.v(0, [(CST_N, 128), (1, CST_N)]))
    dma(VEC.all(), vecs.v(0, [(VEC_N, 128), (1, VEC_N)]))
    dma(fing.all(), rows.v(R_FING, [(0, 128), (1, 1024)]))
    cp(ident.all(), cv(c_ident, 128), eng=POOL)
    mset(ones.all(), 1.0)
    pos_v = V(pos_h, 0, [(1, 32)], 4, pdim=(32, 0, 128))
    dma(pos_v, posT.v(0, [(32, 128), (1, 32)]))
    cp(posf.all(), pos_v)
    for s in range(32):
        ts(ang[s], cv(c_invf, 64), posf[s:s + 1], ALU.mult)
    ts(kk.all(), ang.all(), 1.0 / TWO_PI, ALU.mult, MAGIC, ALU.add)
    ts(kk.all(), kk.all(), -MAGIC, ALU.add)
    stt(ang.all(), kk.all(), -C1, ang.all(), ALU.mult, ALU.add)
    stt(ang.all(), kk.all(), -C2, ang.all(), ALU.mult, ALU.add)
    ts(kk.all(), ang.all(), math.pi / 2, ALU.is_gt, -TWO_PI, ALU.mult)
    stt(kk.all(), ang.all(), math.pi / 2, kk.all(), ALU.add, ALU.add)
    ts(ang.all(), ang.all(), -3.141592, ALU.max, 3.141592, ALU.min)
    ts(kk.all(), kk.all(), -3.141592, ALU.max, 3.141592, ALU.min)
    act(sinT.all(), ang.all(), AF.Sin)
    act(cosT.all(), kk.all(), AF.Sin)

    def w2d(dr, l, K, C, r0, c0, nk, ncol):
        return dr.v(l * K * C + r0 * C + c0, [(C, 128), (128 * C, nk), (1, ncol)])

    def blocks_for(l):
        B = {}

        def colblk(dr, K, C, c0):
            return [(None, w2d(dr, l, K, C, 0, c0, 8, 512))]
        B[0] = colblk(w_in, D, IN_COLS, OFF_Q)
        B[1] = colblk(w_in, D, IN_COLS, OFF_K)
        B[2] = colblk(w_in, D, IN_COLS, OFF_V)
        B[3] = colblk(w_in, D, IN_COLS, OFF_V + 512)
        B[4] = colblk(w_in, D, IN_COLS, OFF_G)
        B[5] = colblk(w_in, D, IN_COLS, OFF_G + 512)
        B[6] = colblk(w_in, D, IN_COLS, OFF_POOL)
        B[7] = colblk(w_in, D, IN_COLS, OFF_XQ)
        for hf in range(2):
            for b in range(3):
                B[8 + 5 * hf + b] = colblk(w_in, D, IN_COLS, OFF_GATE + b * 1024 + hf * 512)
            B[11 + 5 * hf] = [((0, 4), w2d(w_up_pool, l, 512, D, 0, hf * 512, 4, 512)),
                              ((4, 4), w2d(w_up_mem, l, 512, D, 0, hf * 512, 4, 512))]
            B[12 + 5 * hf] = colblk(w_up_ret, 1024, D, hf * 512)
            B[18 + hf] = colblk(w_out, D, D, hf * 512)
        for b in range(8):
            B[20 + b] = colblk(w_mlp1, D, 4096, b * 512)
        for hf in range(2):
            for fb in range(4):
                B[28 + hf * 4 + fb] = [(None, w2d(w_mlp2, l, 4096, D, fb * 1024, hf * 512, 8, 512))]
        B[36] = colblk(w_mem_kv, D, 1024, 0)
        B[37] = colblk(w_mem_kv, D, 1024, 512)
        return B

    def wq_blk(l, b):
        return wq.v((l * NBLK + b) * 128 * 4096, [(4096, 128), (512, 8), (1, 512)])

    def cast_blocks(l, ids):
        B = blocks_for(l)
        for b in ids:
            cast_done.add((l, b))
            for part, src in B[b]:
                base = (l * NBLK + b) * 128 * 4096
                if part is None:
                    dst = wq.v(base, [(4096, 128), (512, 8), (1, 512)])
                else:
                    dst = wq.v(base + part[0] * 512, [(4096, 128), (512, part[1]), (1, 512)])
                dma(dst, src, eng=POOL)

    PRE_IDS = [1, 2, 3, 6]
    ALL_IDS = [36, 37] + list(range(36))
    REST_IDS = [b for b in ALL_IDS if b not in PRE_IDS]
    castq = []
    cast_done = set()

    def pump(n):
        for _ in range(min(n, len(castq))):
            l, b = castq.pop(0)
            cast_blocks(l, [b])

    state = {"ring": 0, "ps": 0}

    def ring_load(l, b):
        assert (l, b) in cast_done, (l, b)
        slot = ring[state["ring"] % RING]
        state["ring"] += 1
        dma(A16.v(slot.off, [(512, 8), (1, 512)]), wq_blk(l, b))
        return slot

    def ps_next():
        a = PS[state["ps"] % 2]
        state["ps"] += 1
        return a

    gvec = lambda base, l: Tl(A32, VEC.off + base + l * 8, [8])
    pscale = lambda l: Tl(A32, VEC.off + V_PSC + l * 4, [4])

    def rms_rstd(src_rows, n, dim):
        for i, v in enumerate(src_rows):
            act(junk[0:dim], v, AF.Square, accum=ss[i:i + 1])
        ts(rstd[0:n], ss[0:n], 1.0 / dim, ALU.mult, EPS, ALU.add, eng=POOL)
        tt(rstd[0:n], rstd[0:n], cv(c_neghalf, n), ALU.pow, eng=POOL)

    def norm_to_hT(gv):
        rms_rstd([X[st] for st in range(NST)], NST, D)
        for st in range(NST):
            ts(hn[st], X[st], rstd[st:st + 1], ALU.mult)
        for kc in range(8):
            pb = PB[kc % 2]
            for st in range(NST):
                tp(pb.v(st * 128, [(1, 128)]), hn[st, kc * 128:(kc + 1) * 128])
            if kc % 2:
                ts(hT[kc], pb.v(0, [(1, 512)]), gv[kc:kc + 1], ALU.mult)
            else:
                act(hT[kc], pb.v(0, [(1, 512)]), AF.Copy, scale=gv[kc:kc + 1])

    def load_x(src, t, buf):
        for st in range(NST):
            dma(buf[st], src.v((t * T + st * 128) * D, [(D, 128), (1, D)]))

    def proj_tok(blk, st, evac):
        ps = ps_next()
        for kc in range(8):
            mm(ps.v(0, [(1, 512)]), hT[kc, st * 128:(st + 1) * 128], blk[kc], start=(kc == 0), stop=(kc == 7))
        evac(ps)

    def proj_feat(blk, j, evac, nk=8, rhs=None, kofs=0):
        ps = ps_next()
        rhs = rhs or hT
        for kc in range(nk):
            mm(ps.v(0, [(1, 512)]), blk[kofs + kc, j * 128:(j + 1) * 128], rhs[kc], start=(kc == 0), stop=(kc == nk - 1))
        evac(ps)

    def rotary(ps, gst, dst, kscale):
        src4 = ps.v(0, [(64, 8), (1, 64)])
        cosb = A32.v(cosT.off + gst * 64, [(0, 8), (1, 64)])
        sinb = A32.v(sinT.off + gst * 64, [(0, 8), (1, 64)])
        tt(A32.v(rotA.off, [(64, 8), (1, 64)]), src4, cosb, ALU.mult)
        tt(A32.v(rotB.off, [(64, 8), (1, 64)]), src4, sinb, ALU.mult)
        a1 = A32.v(rotA.off, [(128, 4), (1, 64)])
        a2 = A32.v(rotA.off + 64, [(128, 4), (1, 64)])
        b1 = A32.v(rotB.off, [(128, 4), (1, 64)])
        b2 = A32.v(rotB.off + 64, [(128, 4), (1, 64)])
        if kscale:
            o1 = A32.v(rotR.off, [(128, 4), (1, 64)])
            o2 = A32.v(rotR.off + 64, [(128, 4), (1, 64)])
        else:
            o1 = A16.v(dst.off, [(128, 4), (1, 64)])
            o2 = A16.v(dst.off + 64, [(128, 4), (1, 64)])
        tt(o1, a1, b2, ALU.subtract, eng=POOL)
        tt(o2, a2, b1, ALU.add, eng=POOL)
        if kscale:
            tt(dst.all(), rotR.all(), cv(c_kscale, 512), ALU.mult, eng=POOL)

    def sub(tl, i):
        return Tl(tl.a, tl.off + i * tl.strides[0], tl.shape[1:])

    def kv_update(c, chunk_idx):
        for hp in range(2):
            ps = PS[2 + hp]
            for hh in range(2):
                h = hp * 2 + hh
                mm(ps.v(hh * 256, [(1, 256)]), kpr[c, h * 128:(h + 1) * 128], vv[c, h * 256:(h + 1) * 256])
            for hh in range(2):
                h = hp * 2 + hh
                stt(Z[h], Z[h], GC[h], ps.v(hh * 256, [(1, 256)]), ALU.mult, ALU.add)

    def snap_Sb(c):
        for h in range(H):
            act(Sbs[c][h], Z[h], AF.Copy, scale=GC[h])

    def exchange_out(l, last_U):
        cp(exl[0:1024], Z.all(), eng=POOL)
        cp(A32.v(exl.off + 1024, [(16, 4), (1, 16)]), A32.v(U.off + 512, [(528, 4), (1, 16)]), eng=POOL)
        dma(exs[l].v(0, [(EXN, 128), (1, EXN)]), exl.all())

    def exchange_in(l):
        if fused:
            S.add(POOL, lambda e: e.collective_compute("AllGather", ALU.bypass, [[0, 1], [2, 3], [4, 5], [6, 7]],
                                                       ins=[exs[l].v(0, [(EXN, 128), (1, EXN)]).ap],
                                                       outs=[exg[l].v(0, [(EXN, 256), (1, EXN)]).ap]),
                  [exs[l].v(0, [(EXN, 128), (1, EXN)])], [exg[l].v(0, [(EXN, 256), (1, EXN)])], cc=True)
        dma(exl.all(), exg[l].v(0, [(EXN, 128), (1, EXN)]))
        ts(Z.all(), exl[0:1024], cv(c_isB, 1), ALU.mult)
        ts(halo.all(), exl[1024:1088], cv(c_isB, 1), ALU.mult)

    def mem_pre(l):
        for c in range(2):
            dma(Xb[0][c], mem_in.v(c * 128 * D, [(D, 128), (1, D)]))
        rms_rstd([Xb[0][c] for c in range(2)], 2, D)
        for c in range(2):
            ts(memn[c], Xb[0][c], rstd[c:c + 1], ALU.mult)
        gv = gvec(V_GMEM, l)
        for kc in range(8):
            pb = PB[kc % 2]
            for c in range(2):
                tp(pb.v(c * 128, [(1, 128)]), memn[c, kc * 128:(kc + 1) * 128])
            ts(memnT[kc], pb.v(0, [(1, 256)]), gv[kc:kc + 1], ALU.mult)
        bk = ring_load(l, 36)
        for h in range(H):
            ps = ps_next()
            for kc in range(8):
                mm(ps.v(0, [(1, 256)]), bk[kc, h * 128:(h + 1) * 128], memnT[kc], start=(kc == 0), stop=(kc == 7))
            act(kmT[h], ps.v(0, [(1, 256)]), AF.Copy)
        bv = ring_load(l, 37)
        for c in range(2):
            ps = ps_next()
            for kc in range(8):
                mm(ps.v(0, [(1, 512)]), memnT[kc, c * 128:(c + 1) * 128], bv[kc], start=(kc == 0), stop=(kc == 7))
            act(vm[c], ps.v(0, [(1, 512)]), AF.Copy)

    def prepass(l, src):
        mset(Z.all(), 0.0)
        gv = gvec(V_GMIX, l)
        nxt = None
        for t in range(NT):
            pump(5)
            if t == 0:
                load_x(src, t, X.flip())
            else:
                X.flip()
            norm_to_hT(gv)
            if t + 1 < NT:
                load_x(src, t + 1, Xb[(X.n + 1) % 2])
            bk = ring_load(l, 1)
            for st in range(NST):
                proj_tok(bk, st, lambda ps, st=st: rotary(ps, t * NST + st, sub(kpr, st), True))
            for hf in range(2):
                bv = ring_load(l, 2 + hf)
                for st in range(NST):
                    proj_tok(bv, st, lambda ps, st=st, hf=hf: act(vv[st, hf * 512:(hf + 1) * 512], ps.v(0, [(1, 512)]), AF.Copy))
            for c in range(NST):
                kv_update(c, t * NST + c)
            if t == NT - 1:
                bp = ring_load(l, 6)
                for g in range(4):
                    proj_feat(bp, g, lambda ps, g=g: act(U[g, 16:528], ps.v(0, [(1, 512)]), AF.Copy))
        exchange_out(l, None)

    def main(l, src, dst_x, final):
        gv = gvec(V_GMIX, l)
        gv2 = gvec(V_GMLP, l)
        dma(retg.all(), rows.v(R_RETG + l * 1024, [(0, 128), (1, 1024)]))
        dma(A16.v(poolw.off, [(128, 4), (1, 128)]), pool_w.v(l * 4 * 128 * 128, [(128, 128), (128 * 128, 4), (1, 128)]), eng=POOL)
        for t in range(NT):
            pump(5)
            if t == 0:
                load_x(src, t, X.flip())
            else:
                X.flip()
            norm_to_hT(gv)
            if t + 1 < NT:
                load_x(src, t + 1, Xb[(X.n + 1) % 2])
            bq = ring_load(l, 0)
            for st in range(NST):
                proj_tok(bq, st, lambda ps, st=st: rotary(ps, t * NST + st, sub(qrot, st), False))
            bk = ring_load(l, 1)
            for st in range(NST):
                proj_tok(bk, st, lambda ps, st=st: rotary(ps, t * NST + st, sub(kpr, st), True))
            for hf in range(2):
                bv = ring_load(l, 2 + hf)
                for st in range(NST):
                    proj_tok(bv, st, lambda ps, st=st, hf=hf: act(vv[st, hf * 512:(hf + 1) * 512], ps.v(0, [(1, 512)]), AF.Copy))
            for hf in range(2):
                bg = ring_load(l, 4 + hf)
                for st in range(NST):
                    def ev(ps, st=st, hf=hf):
                        act(gs[st, hf * 512:(hf + 1) * 512], ps.v(0, [(1, 512)]), AF.Silu)
                        tt(gs[st, hf * 512:(hf + 1) * 512], gs[st, hf * 512:(hf + 1) * 512],
                           retg[hf * 512:(hf + 1) * 512], ALU.mult, eng=POOL)
                    proj_tok(bg, st, ev)
            chk("proj", [(qrot, 512), (kpr, 512), (vv, 1024), (gs, 1024)])
            for c in range(NST):
                snap_Sb(c)
                kv_update(c, t * NST + c)
            ysb = Tl(A32, rotA.off, [4, 256])

            def stageA(c):
                par = c % 2
                for h in range(H):
                    tp(PB[0].v(h * 128, [(1, 128)]), qrot[c, h * 128:(h + 1) * 128])
                    tp(PB[1].v(h * 128, [(1, 128)]), kpr[c, h * 128:(h + 1) * 128])
                act(qT[par].all(), PB[0].v(0, [(1, 512)]), AF.Copy)
                cp(kT[par].all(), PB[1].v(0, [(1, 512)]))
                for h in range(H):
                    mm(PS[2].v(h * 128, [(1, 128)]), kT[par][h], qT[par][h])
                tt(scm[par].all(), PS[2].v(0, [(1, 512)]), cv(c_cmask, 512), ALU.mult)

            def stageB(c):
                par = c % 2
                for h in range(H):
                    ps = ps_next()
                    mm(ps.v(0, [(1, 256)]), scm[par][h], vv[c, h * 256:(h + 1) * 256])
                    mm(ps.v(256, [(1, 256)]), qT[par][h], Sbs[c][h])
                    act(ysb[h], ps.v(0, [(1, 256)]), AF.Copy)
                    stt(ysb[h], ysb[h], 1.0, ps.v(256, [(1, 256)]), ALU.mult, ALU.add)
                for h in range(H):
                    act(junk[0:256], ysb[h], AF.Square, accum=ys[h:h + 1])
                tt(ys[4:8], ys[0:4], cv(c_yscale2, 4), ALU.mult, eng=POOL)
                ts(ys[4:8], ys[4:8], 1.0, ALU.mult, EPS, ALU.add, eng=POOL)
                tt(ys[4:8], ys[4:8], cv(c_neghalf, 4), ALU.pow, eng=POOL)
                tt(sc[0:4], ys[4:8], cv(c_yscale, 4), ALU.mult, eng=POOL)
                for h in range(H):
                    ts(ysb[h], ysb[h], sc[h:h + 1], ALU.mult)
                    tt(yret[par][h * 256:(h + 1) * 256], ysb[h], gs[c, h * 256:(h + 1) * 256], ALU.mult, eng=POOL)
                pb = PB[2 + par]
                for kc in range(8):
                    tp(pb.v(kc * 128, [(1, 128)]), yret[par][kc * 128:(kc + 1) * 128])
                act(A16.v(yretT.off + c * 128, [(512, 8), (1, 128)]), pb.v(0, [(128, 8), (1, 128)]), AF.Copy)

            for c in range(NST):
                stageA(c)
                if c >= 1:
                    stageB(c - 1)
            stageB(NST - 1)
            chk("ret", [(yretT, 4096)])
            cp(A32.v(U.off, [(528, 4), (1, 16)]), A32.v(halo.off, [(16, 4), (1, 16)]), eng=POOL)
            bp = ring_load(l, 6)
            for g in range(4):
                proj_feat(bp, g, lambda ps, g=g: act(U[g, 16:528], ps.v(0, [(1, 512)]), AF.Copy))
            cp(A32.v(halo.off, [(16, 4), (1, 16)]), A32.v(U.off + 512, [(528, 4), (1, 16)]), eng=POOL)
            for g in range(4):
                w = 2 << g
                cur, curlen, shift = sub(U, g), 528, 1
                bufs = [tmpA, tmpB]
                bi = 0
                while shift < w:
                    nl = curlen - shift
                    nxt = bufs[bi]
                    tt(nxt[0:nl], cur[shift:curlen], cur[0:nl], ALU.add, eng=POOL)
                    cur, curlen = nxt, nl
                    bi ^= 1
                    shift *= 2
                o = 17 - w
                stt(dd[g], cur[o:o + 512], 1.0 / w, U[g, 16:528], ALU.mult, ALU.subtract)
                if t == 0:
                    tt(mtmp[0, 0:16], cur[o:o + 16], cv(c_invcnt + g * 16, 16), ALU.mult)
                    tt(dd[g, 0:16], mtmp[0, 0:16], U[g, 16:32], ALU.subtract)
            psc = pscale(l)
            for g in range(4):
                ps = ps_next()
                mm(ps.v(0, [(1, 512)]), poolw[g], dd[g])
                act(ypoolT[g], ps.v(0, [(1, 512)]), AF.Copy, scale=psc[g:g + 1])
            chk("pool", [(ypoolT, 2048)])
            bx = ring_load(l, 7)
            for h in range(H):
                proj_feat(bx, h, lambda ps, h=h: act(xqT[h], ps.v(0, [(1, 512)]), AF.Copy))
            for h in range(H):
                par = h % 2
                for c in range(2):
                    ps = ps_next()
                    mm(ps.v(0, [(1, 512)]), kmT[h, c * 128:(c + 1) * 128], xqT[h])
                    act(pT[par][c], ps.v(0, [(1, 512)]), AF.Exp, scale=128 ** -0.5)
                pso = ps_next()
                for c in range(2):
                    mm(pso.v(0, [(1, 512)]), vm[c, h * 128:(h + 1) * 128], pT[par][c], start=(c == 0), stop=(c == 1))
                for c in range(2):
                    mm(PS[2].v(0, [(1, 512)]), ones.all(), pT[par][c], start=(c == 0), stop=(c == 1))
                recip(rsum.all(), PS[2].v(0, [(1, 512)]))
                tt(ymemT[h], pso.v(0, [(1, 512)]), rsum.all(), ALU.mult)
            chk("mem", [(ymemT, 2048)])
            for hf in range(2):
                for b in range(3):
                    bgate = ring_load(l, 8 + 5 * hf + b)
                    for j in range(4):
                        proj_feat(bgate, j, lambda ps, b=b, j=j: act(sg[b, j], ps.v(0, [(1, 512)]), AF.Sigmoid))
                bpm = ring_load(l, 11 + 5 * hf)
                bret = ring_load(l, 12 + 5 * hf)
                for j in range(4):
                    proj_feat(bpm, j, lambda ps, j=j: tt(mtmp[0], ps.v(0, [(1, 512)]), sg[0, j], ALU.mult), nk=4, rhs=ypoolT)
                    proj_feat(bret, j, lambda ps, j=j: tt(mtmp[1], ps.v(0, [(1, 512)]), sg[1, j], ALU.mult), nk=8, rhs=yretT)
                    tt(mtmp[0], mtmp[0], mtmp[1], ALU.add, eng=POOL)
                    proj_feat(bpm, j, lambda ps, j=j: tt(mtmp[1], ps.v(0, [(1, 512)]), sg[2, j], ALU.mult), nk=4, rhs=ymemT, kofs=4)
                    tt(mergedT[hf * 4 + j], mtmp[0], mtmp[1], ALU.add, eng=POOL)
            chk("merge", [(mergedT, 4096)])
            for hf in range(2):
                bo = ring_load(l, 18 + hf)
                for st in range(NST):
                    ps = ps_next()
                    for kc in range(8):
                        mm(ps.v(0, [(1, 512)]), mergedT[kc, st * 128:(st + 1) * 128], bo[kc], start=(kc == 0), stop=(kc == 7))
                    tt(X[st, hf * 512:(hf + 1) * 512], ps.v(0, [(1, 512)]), X[st, hf * 512:(hf + 1) * 512], ALU.add)
            chk("out", [(X.cur, 4096)])
            norm_to_hT(gv2)
            for b in range(8):
                b1 = ring_load(l, 20 + b)
                for j in range(4):
                    def ev(ps, b=b, j=j):
                        act(mtmp[j % 2], ps.v(0, [(1, 512)]), AF.Relu)
                        tt(hid[b * 4 + j], mtmp[j % 2], mtmp[j % 2], ALU.mult, eng=POOL)
                    proj_feat(b1, j, ev)
            for hf in range(2):
                b2s = [ring_load(l, 28 + hf * 4 + fb) for fb in range(4)]
                for st in range(NST):
                    ps = ps_next()
                    for fb in range(4):
                        for k8 in range(8):
                            mm(ps.v(0, [(1, 512)]), hid[fb * 8 + k8, st * 128:(st + 1) * 128], b2s[fb][k8],
                               start=(fb == 0 and k8 == 0), stop=(fb == 3 and k8 == 7))
                    tt(X[st, hf * 512:(hf + 1) * 512], ps.v(0, [(1, 512)]), X[st, hf * 512:(hf + 1) * 512], ALU.add)
            chk("mlp", [(X.cur, 4096)])
            if final:
                rms_rstd([X[st] for st in range(NST)], NST, D)
                for st in range(NST):
                    stt(X[st], X[st], rstd[st:st + 1], fing.all(), ALU.mult, ALU.mult)
            for st in range(NST):
                dma(dst_x.v((t * T + st * 128) * D, [(D, 128), (1, D)]), X[st])

    def _program():
        if do_pre0 and not fused:
            cast_blocks(0, PRE_IDS)
            prepass(0, x_in)
        if fused:
            cast_blocks(0, PRE_IDS)
            castq.extend([(0, b) for b in REST_IDS] + [(1, b) for b in PRE_IDS] + [(1, b) for b in REST_IDS])
            prepass(0, x_in)
            pump(len(REST_IDS) + len(PRE_IDS) - 40 if len(REST_IDS) + len(PRE_IDS) > 40 else 0)
            while castq and castq[0][0] == 0:
                pump(1)
            exchange_in(0)
            mem_pre(0)
            main(0, x_in, xs, False)
            while castq and castq[0] in [(1, b) for b in PRE_IDS]:
                pump(1)
            prepass(1, xs)
            pump(len(castq))
            exchange_in(1)
            mem_pre(1)
            main(1, xs, out_d, True)
            return
        if do_main0:
            cast_blocks(0, ALL_IDS)
            cast_blocks(1, PRE_IDS)
            exchange_in(0)
            mem_pre(0)
            main(0, x_in, xs, False)
            prepass(1, xs)
        if do_main1:
            cast_blocks(1, ALL_IDS)
            exchange_in(1)
            mem_pre(1)
            main(1, xs, out_d, True)

    try:
        _program()
    except _Stop:
        pass
    S.emit(nc)
    return nc


def _consts(core):
    half = core % 2
    c = np.zeros((128, CST_N), np.float32)
    j = np.arange(128)
    c[:, c_ident:c_ident + 128] = np.eye(128, dtype=np.float32)
    cm = (j[None, :] >= j[:, None]).astype(np.float32)
    c[:, c_cmask:c_cmask + 512] = np.tile(cm, (1, 4))
    for h in range(H):
        g = np.float64(GAMMA[h])
        c[:, c_kscale + h * 128:c_kscale + (h + 1) * 128] = (g ** (-(j + 1.0)) * 128 ** -0.5)[:, None]
        c[:, c_yscale + h] = g ** (j + 1.0)
        c[:, c_yscale2 + h] = g ** (2.0 * (j + 1.0)) / 256.0
        c[:, c_gcb + h] = g ** 128 * half
    c[:, c_invf:c_invf + 64] = (10000.0 ** (-np.arange(64, dtype=np.float64) / 64.0)).astype(np.float32)[None, :]
    for gi, w in enumerate((2, 4, 8, 16)):
        tt_ = np.arange(16)
        cnt = np.minimum(tt_ + 1, w) if half == 0 else np.full(16, w)
        c[:, c_invcnt + gi * 16:c_invcnt + (gi + 1) * 16] = (1.0 / cnt)[None, :]
    c[:, c_isB] = float(half)
    c[:, c_neghalf:c_neghalf + 8] = -0.5
    return c


def _in_map(core, inp):
    b, half = core // 2, core % 2
    sl = slice(half * TOK, (half + 1) * TOK)
    f = lambda a: np.ascontiguousarray(a, dtype=np.float32)
    vec = np.zeros((128, VEC_N), np.float32)
    for l in range(2):
        vec[:, V_GMIX + l * 8:V_GMIX + l * 8 + 8] = inp["norm_mix_g"][l].reshape(8, 128).T
        vec[:, V_GMLP + l * 8:V_GMLP + l * 8 + 8] = inp["norm_mlp_g"][l].reshape(8, 128).T
        vec[:, V_GMEM + l * 8:V_GMEM + l * 8 + 8] = inp["mem_norm_g"][l].reshape(8, 128).T
        vec[:, V_PSC + l * 4:V_PSC + l * 4 + 4] = inp["pool_scale"][l].reshape(4, 128).T
    rows = np.concatenate([inp["ret_norm_g"][0], inp["ret_norm_g"][1], inp["final_norm_g"]]).astype(np.float32)
    return {
        "x": f(inp["x"][b, sl]),
        "mem": f(inp["mem"][b]),
        "posT": np.ascontiguousarray(np.asarray(inp["positions"])[b, sl].reshape(32, 128).T.astype(np.int32)),
        "cst": _consts(core),
        "vecs": vec,
        "rows": rows,
        "w_in": f(inp["w_in"]), "pool_w": f(inp["pool_w"]), "w_mem_kv": f(inp["w_mem_kv"]),
        "w_up_pool": f(inp["w_up_pool"]), "w_up_ret": f(inp["w_up_ret"]), "w_up_mem": f(inp["w_up_mem"]),
        "w_out": f(inp["w_out"]), "w_mlp1": f(inp["w_mlp1"]), "w_mlp2": f(inp["w_mlp2"]),
    }


FUSED = True


def kernel(**inputs):
    inp = {k: np.asarray(v) for k, v in inputs.items()}
    maps = [_in_map(c, inp) for c in range(NCORES)]
    cores = list(range(NCORES))
    if FUSED:
        nc = build("fused")
        res = run_bass_kernel_spmd(nc, maps, core_ids=cores)
        outs = [r["out"] for r in res.results]
    else:
        def partner(arrs):
            return [arrs[c - 1] if c % 2 else arrs[c] for c in range(NCORES)]
        r0 = run_bass_kernel_spmd(build("unfused", 0), maps, core_ids=cores).results
        ex0 = partner([r["exs0"] for r in r0])
        m1 = [dict(m, exg0=ex0[c]) for c, m in enumerate(maps)]
        r1 = run_bass_kernel_spmd(build("unfused", 1), m1, core_ids=cores).results
        ex1 = partner([r["exs1"] for r in r1])
        m2 = [dict(m, exg1=ex1[c], xs=r1[c]["xs"]) for c, m in enumerate(maps)]
        r2 = run_bass_kernel_spmd(build("unfused", 2), m2, core_ids=cores).results
        outs = [r["out"] for r in r2]
    out = np.empty((4, 8192, D), np.float32)
    for c in range(NCORES):
        out[c // 2, (c % 2) * TOK:(c % 2 + 1) * TOK] = outs[c]
    return out
```

```python
import math
import numpy as np
import concourse.bass as bass
import concourse.mybir as mybir
from concourse.bass_utils import run_bass_kernel_spmd

F32 = mybir.dt.float32
BF16 = mybir.dt.bfloat16
I32 = mybir.dt.int32
AF = mybir.ActivationFunctionType
ALU = mybir.AluOpType

NCORES = 8
D = 1024
TOK = 4096
T = 512
NT = TOK // T
NST = 4
H = 4
IN_COLS = 7168
OFF_POOL, OFF_Q, OFF_K, OFF_V, OFF_G, OFF_XQ, OFF_GATE = 0, 512, 1024, 1536, 2560, 3584, 4096
EPS = 1e-6
GAMMA = [1.0 - 2.0 ** (-5.0 - h) for h in range(H)]
GC = [g ** 128 for g in GAMMA]
NBLK = 38
RING = 4
MAGIC = 12582912.0
TWO_PI = 2.0 * math.pi
C1 = 6.28125
C2 = TWO_PI - C1
EXN = 1024 + 64

c_ident = 0
c_cmask = c_ident + 128
c_kscale = c_cmask + 512
c_yscale = c_kscale + 512
c_yscale2 = c_yscale + 4
c_invf = c_yscale2 + 4
c_invcnt = c_invf + 64
c_isB = c_invcnt + 64
c_neghalf = c_isB + 1
c_gcb = c_neghalf + 8
CST_N = c_gcb + 4
V_GMIX, V_GMLP, V_GMEM, V_PSC = 0, 16, 32, 48
VEC_N = 56
R_RETG, R_FING = 0, 2048
ROW_N = 3072


class V:
    __slots__ = ("ap", "key", "lo", "hi")

    def __init__(self, handle, off, dims, esz, pdim=None, track=True):
        if pdim is not None:
            F, p0, np_ = pdim
            self.ap = bass.AP(handle, p0 * F + off, [[F, np_]] + [[s, n] for s, n in dims])
        else:
            self.ap = bass.AP(handle, off, [[s, n] for s, n in dims])
        lo = off + sum(min(0, s * (n - 1)) for s, n in dims)
        hi = off + sum(max(0, s * (n - 1)) for s, n in dims) + 1
        self.key = handle.name if track else None
        self.lo, self.hi = lo * esz, hi * esz


class Arena:
    def __init__(self, nc, name, nelem, dtype, esz, psum=False):
        self.F = nelem
        self.esz = esz
        self.h = (nc.alloc_psum_tensor if psum else nc.alloc_sbuf_tensor)(name, [128, nelem], dtype)
        self.ptr = 0

    def alloc(self, n):
        o = self.ptr
        self.ptr += n
        assert self.ptr <= self.F, (self.h.name, self.ptr, self.F)
        return o

    def v(self, off, dims, p0=0, np_=128):
        return V(self.h, off, dims, self.esz, pdim=(self.F, p0, np_))


class Tl:
    def __init__(self, arena, off, shape):
        self.a, self.off, self.shape = arena, off, tuple(shape)
        st = []
        acc = 1
        for n in reversed(self.shape):
            st.append(acc)
            acc *= n
        self.strides = tuple(reversed(st))
        self.size = acc

    def __getitem__(self, idx):
        if not isinstance(idx, tuple):
            idx = (idx,)
        off = self.off
        dims = []
        for i, n in enumerate(self.shape):
            s = self.strides[i]
            ix = idx[i] if i < len(idx) else slice(None)
            if isinstance(ix, int):
                off += ix * s
            else:
                a = ix.start or 0
                b = n if ix.stop is None else ix.stop
                off += a * s
                dims.append((s, b - a))
        merged = []
        for s, n in dims:
            if merged and merged[-1][0] == s * n:
                merged[-1] = (s, merged[-1][1] * n)
            else:
                merged.append((s, n))
        if not merged:
            merged = [(1, 1)]
        return self.a.v(off, merged)

    def all(self):
        return self[tuple(slice(None) for _ in self.shape)]


class Op:
    __slots__ = ("eng", "fn", "deps", "dma", "sig", "needed")

    def __init__(self, eng, fn, dma):
        self.eng, self.fn, self.dma = eng, fn, dma
        self.deps = set()
        self.sig = None
        self.needed = False


class Sched:
    ENGS = ("pe", "act", "dve", "pool", "sp")

    def __init__(self, ndma=8):
        self.ops = {e: [] for e in self.ENGS}
        self.track = {}
        self.ndma = ndma
        self.dma_ops = {e: [] for e in self.ENGS}

    def _access(self, v, op, is_write):
        if v.key is None:
            return
        segs = self.track.get(v.key, [])
        lo, hi = v.lo, v.hi
        if is_write and op.eng == "pe" and v.key[:2] in ("ps", "pb"):
            lo, hi = 0, 2048
        out = []
        cover = []
        for sg in segs:
            slo, shi, w, rs = sg
            if shi <= lo or slo >= hi:
                out.append(sg)
                continue
            if slo < lo:
                out.append([slo, lo, w, list(rs)])
            if shi > hi:
                out.append([hi, shi, w, list(rs)])
            cover.append([max(slo, lo), min(shi, hi), w, rs])
        for c in cover:
            if c[2] is not None:
                self._dep(op, c[2], raw=not is_write)
            if is_write:
                for r in c[3]:
                    self._dep(op, r, raw=False)
        if is_write:
            out.append([lo, hi, op, []])
        else:
            cover.sort(key=lambda c: c[0])
            pos = lo
            for c in cover:
                if c[0] > pos:
                    out.append([pos, c[0], None, [op]])
                out.append([c[0], c[1], c[2], list(c[3]) + [op]])
                pos = c[1]
            if pos < hi:
                out.append([pos, hi, None, [op]])
        self.track[v.key] = out

    def _dep(self, op, other, raw):
        if other is op:
            return
        if other.eng == op.eng == "pe" and not other.dma and not op.dma:
            return
        op.deps.add(other)

    def add(self, eng, fn, reads=(), writes=(), dma=False, cc=False):
        op = Op(eng, fn, dma or cc)
        self.cc_ops = getattr(self, "cc_ops", [])
        for v in reads:
            self._access(v, op, False)
        for v in writes:
            self._access(v, op, True)
        if cc:
            self.cc_ops.append(op)
        if dma:
            lst = self.dma_ops[eng]
            if len(lst) >= self.ndma:
                op.deps.add(lst[len(lst) - self.ndma])
            lst.append(op)
        self.ops[eng].append(op)
        return op

    def emit(self, nc):
        for e in self.ENGS:
            for op in self.ops[e]:
                for d in op.deps:
                    d.needed = True
        import contextlib
        with contextlib.ExitStack() as es:
            esem = {e: es.enter_context(nc.semaphore("s_" + e)) for e in self.ENGS}
            dsem = {e: [es.enter_context(nc.semaphore("d_%s%d" % (e, i))) for i in range(self.ndma)]
                    for e in self.ENGS if self.dma_ops[e]}
            ccs = getattr(self, "cc_ops", [])
            ccsem = es.enter_context(nc.semaphore("s_cc")) if ccs else None
            for i, op in enumerate(ccs):
                op.sig = (ccsem, i + 1, 1)
            for e in self.ENGS:
                cnt = 0
                dcnt = 0
                for op in self.ops[e]:
                    if op.sig is not None:
                        continue
                    if op.dma:
                        k = dcnt % self.ndma
                        op.sig = (dsem[e][k], 16 * (dcnt // self.ndma + 1), 16)
                        dcnt += 1
                    elif op.needed:
                        cnt += 1
                        op.sig = (esem[e], cnt, 1)
            block = es.enter_context(nc.Block())
            engmap = {"pe": block.tensor, "act": block.scalar, "dve": block.vector, "pool": block.gpsimd,
                      "sp": block.sync}
            final_dma = [op for e in self.ENGS for op in self.dma_ops[e]]
            for e in self.ENGS:
                ops = self.ops[e]
                last = e == "sp"

                def body(eng, ops=ops, last=last):
                    waited = {}
                    for op in ops:
                        for d in sorted(op.deps, key=lambda d: d.sig[1]):
                            sem, val, _ = d.sig
                            if waited.get(id(sem), 0) < val:
                                eng.wait_ge(sem, val)
                                waited[id(sem)] = val
                        ins = op.fn(eng)
                        if op.sig is not None:
                            ins.then_inc(op.sig[0], op.sig[2])
                    if last:
                        for q in self.ENGS:
                            lst = self.dma_ops[q]
                            for op in lst[-self.ndma:]:
                                sem, val, _ = op.sig
                                if waited.get(id(sem), 0) < val:
                                    eng.wait_ge(sem, val)
                                    waited[id(sem)] = val

                engmap[e](body)


class DR:
    def __init__(self, nc, name, shape, dtype, kind, esz, track=True):
        self.h = nc.dram_tensor(name, list(shape), dtype, kind=kind)
        self.esz = esz
        self.track = track

    def v(self, off, dims):
        return V(self.h, off, dims, self.esz, pdim=None, track=self.track)


class _Stop(Exception):
    pass


def build(mode="fused", stage=0, debug=None):
    nc = bass.Bass("TRN2", target_bir_lowering=False)
    fused = mode == "fused"
    S = Sched()
    IN, OUT, INT = "ExternalInput", "ExternalOutput", "Internal"

    do_pre0 = fused or stage == 0
    do_main0 = fused or stage == 1
    do_pre1 = fused or stage == 1
    do_main1 = fused or stage == 2

    x_in = DR(nc, "x", [TOK, D], F32, IN, 4, track=False)
    mem_in = DR(nc, "mem", [256, D], F32, IN, 4, track=False)
    posT = DR(nc, "posT", [128, 32], I32, IN, 4, track=False)
    cst = DR(nc, "cst", [128, CST_N], F32, IN, 4, track=False)
    vecs = DR(nc, "vecs", [128, VEC_N], F32, IN, 4, track=False)
    rows = DR(nc, "rows", [ROW_N], F32, IN, 4, track=False)
    w_in = DR(nc, "w_in", [2, D, IN_COLS], F32, IN, 4, track=False)
    pool_w = DR(nc, "pool_w", [2, 4, 128, 128], F32, IN, 4, track=False)
    w_mem_kv = DR(nc, "w_mem_kv", [2, D, 1024], F32, IN, 4, track=False)
    w_up_pool = DR(nc, "w_up_pool", [2, 512, D], F32, IN, 4, track=False)
    w_up_ret = DR(nc, "w_up_ret", [2, 1024, D], F32, IN, 4, track=False)
    w_up_mem = DR(nc, "w_up_mem", [2, 512, D], F32, IN, 4, track=False)
    w_out = DR(nc, "w_out", [2, D, D], F32, IN, 4, track=False)
    w_mlp1 = DR(nc, "w_mlp1", [2, D, 4096], F32, IN, 4, track=False)
    w_mlp2 = DR(nc, "w_mlp2", [2, 4096, D], F32, IN, 4, track=False)
    wq = DR(nc, "wq", [2, NBLK, 128, 4096], BF16, INT, 2)
    if fused:
        out_d = DR(nc, "out", [TOK, D], F32, OUT, 4)
        xs = DR(nc, "xs", [TOK, D], F32, INT, 4)
        exs = [DR(nc, "exs%d" % l, [128, EXN], F32, INT, 4) for l in range(2)]
        exg = [DR(nc, "exg%d" % l, [256, EXN], F32, INT, 4) for l in range(2)]
    else:
        out_d = DR(nc, "out", [TOK, D], F32, OUT, 4) if stage == 2 else None
        xs = DR(nc, "xs", [TOK, D], F32, OUT if stage == 1 else IN, 4, track=(stage == 1)) if stage >= 1 else None
        exs = [None, None]
        exg = [None, None]
        if stage == 0:
            exs[0] = DR(nc, "exs0", [128, EXN], F32, OUT, 4)
        if stage == 1:
            exg[0] = DR(nc, "exg0", [128, EXN], F32, IN, 4, track=False)
            exs[1] = DR(nc, "exs1", [128, EXN], F32, OUT, 4)
        if stage == 2:
            exg[1] = DR(nc, "exg1", [128, EXN], F32, IN, 4, track=False)
    dbg = DR(nc, "dbg", [128, 4096], F32, OUT, 4) if debug else None

    A32 = Arena(nc, "a32", 22700, F32, 4)
    A16 = Arena(nc, "a16", 56200, BF16, 2)
    PS = [Arena(nc, "ps%d" % i, 512, F32, 4, psum=True) for i in range(6)]
    PB = [Arena(nc, "pb%d" % i, 1024, BF16, 2, psum=True) for i in range(2)]
    pos_h = nc.alloc_sbuf_tensor("pos_i", [128, 32], I32)

    def t32(shape):
        return Tl(A32, A32.alloc(int(np.prod(shape))), shape)

    def t16(shape):
        return Tl(A16, A16.alloc(int(np.prod(shape))), shape)

    cosT = t32([32, 64])
    sinT = t32([32, 64])
    CST = t32([CST_N])
    VEC = t32([VEC_N])
    retg = t32([1024])
    fing = t32([1024])
    Z = t32([H, 256])
    Xb = [t32([NST, 1024]), t32([NST, 1024])]

    class _XRef:
        cur = Xb[0]
        n = 0

        def __getitem__(self, idx):
            return self.cur[idx]

        def flip(self):
            _XRef.n += 1
            _XRef.cur = Xb[_XRef.n % 2]
            return _XRef.cur
    X = _XRef()
    ss = t32([8])
    rstd = t32([8])
    ys = t32([8])
    sc = t32([8])
    halo = t32([4, 16])
    tr32 = A32.ptr
    U = t32([4, 528])
    tmpA = t32([528])
    tmpB = t32([528])
    mtmp = t32([2, 512])
    rsum = t32([512])
    end_a = A32.ptr
    A32.ptr = tr32
    rotA = t32([512])
    rotB = t32([512])
    rotR = t32([512])
    posf = t32([32])
    A32.ptr = max(A32.ptr, end_a)
    exl = t32([EXN])
    ang = Tl(A32, Xb[0].off, [32, 64])
    kk = Tl(A32, Xb[0].off + 2048, [32, 64])
    ident = t16([128])
    ones = t16([128])
    Sb = t16([H, 256])
    kmT = t16([H, 256])
    vm = t16([2, 512])
    poolw = t16([4, 128])
    ring = [t16([8, 512]) for _ in range(RING)]
    hT = t16([8, 512])
    hn = t16([NST, 1024])
    junk = t16([1024])
    tr16 = A16.ptr
    mergedT = t16([8, 512])
    yretT = t16([8, 512])
    ph = A16.ptr
    qrot = t16([NST, 512])
    kpr = t16([NST, 512])
    vv = t16([NST, 1024])
    gs = t16([NST, 1024])
    qT = [t16([H, 128]) for _ in range(2)]
    kT = [t16([H, 128]) for _ in range(2)]
    scm = [t16([H, 128]) for _ in range(2)]
    yret = [t16([1024]) for _ in range(2)]
    end_r = A16.ptr
    A16.ptr = ph
    dd = t16([4, 512])
    ypoolT = t16([4, 512])
    xqT = t16([4, 512])
    pT = [t16([2, 512]) for _ in range(2)]
    ymemT = t16([4, 512])
    sg = t16([3, 4, 512])
    end_p = A16.ptr
    A16.ptr = tr16 + 4096
    A16.ptr = tr16
    hid = t16([32, 512])
    memn = Tl(A16, hid.off, [2, 1024])
    memnT = Tl(A16, hid.off + 2048, [8, 256])
    A16.ptr = max(end_r, end_p, A16.ptr)

    PE, ACT, DVE, POOL, SP = "pe", "act", "dve", "pool", "sp"

    def mm(out, lhsT, rhs, start=True, stop=True):
        return S.add(PE, lambda e: e.matmul(out.ap, lhsT.ap, rhs.ap, start=start, stop=stop), [lhsT, rhs], [out])

    idv = ident.all()

    def tp(out, in_):
        return S.add(PE, lambda e: e.transpose(out.ap, in_.ap, idv.ap), [in_, idv], [out])

    def act(out, in_, func, scale=None, bias=None, accum=None):
        rd = [in_]
        kw = {}
        for nm, val in (("scale", scale), ("bias", bias)):
            if val is not None:
                if isinstance(val, V):
                    rd.append(val)
                    kw[nm] = val.ap
                else:
                    kw[nm] = float(val)
        wr = [out]
        if accum is not None:
            wr.append(accum)
            kw["accum_out"] = accum.ap
        return S.add(ACT, lambda e: e.activation(out.ap, in_.ap, func, **kw), rd, wr)

    def tt(out, in0, in1, op, eng=DVE):
        return S.add(eng, lambda e: e.tensor_tensor(out.ap, in0.ap, in1.ap, op), [in0, in1], [out])

    def ts(out, in0, s1, op0, s2=None, op1=None, eng=DVE):
        rd = [in0]
        a1 = s1.ap if isinstance(s1, V) else float(s1)
        if isinstance(s1, V):
            rd.append(s1)
        if op1 is None:
            return S.add(eng, lambda e: e.tensor_scalar(out.ap, in0.ap, a1, None, op0), rd, [out])
        a2 = s2.ap if isinstance(s2, V) else float(s2)
        if isinstance(s2, V):
            rd.append(s2)
        return S.add(eng, lambda e: e.tensor_scalar(out.ap, in0.ap, a1, a2, op0, op1), rd, [out])

    def stt(out, in0, scl, in1, op0, op1):
        rd = [in0, in1]
        a = scl.ap if isinstance(scl, V) else float(scl)
        if isinstance(scl, V):
            rd.append(scl)
        return S.add(DVE, lambda e: e.scalar_tensor_tensor(out.ap, in0.ap, a, in1.ap, op0, op1), rd, [out])

    def cp(out, in_, eng=DVE):
        return S.add(eng, lambda e: e.tensor_copy(out.ap, in_.ap), [in_], [out])

    def recip(out, in_):
        return S.add(DVE, lambda e: e.reciprocal(out.ap, in_.ap), [in_], [out])

    def mset(out, val, eng=POOL):
        return S.add(eng, lambda e: e.memset(out.ap, val), [], [out])

    def dma(out, in_, eng=SP):
        return S.add(eng, lambda e: e.dma_start(out=out.ap, in_=in_.ap), [in_], [out], dma=True)

    def cv(o, n):
        return CST[o:o + n]

    def chk(name, tiles):
        if debug != name:
            return
        o = 0
        for tl, n in tiles:
            dma(dbg.v(o, [(4096, 128), (1, n)]), tl.a.v(tl.off, [(1, n)]), eng=POOL)
            o += n
        raise _Stop()

    dma(CST.all(), cst.v(0, [(CST_N, 128), (1, CST_N)]))
    dma(VEC.all(), vecs.v(0, [(VEC_N, 128), (1, VEC_N)]))
    dma(fing.all(), rows.v(R_FING, [(0, 128), (1, 1024)]))
    cp(ident.all(), cv(c_ident, 128), eng=POOL)
    mset(ones.all(), 1.0)
    pos_v = V(pos_h, 0, [(1, 32)], 4, pdim=(32, 0, 128))
    dma(pos_v, posT.v(0, [(32, 128), (1, 32)]))
    cp(posf.all(), pos_v)
    for s in range(32):
        ts(ang[s], cv(c_invf, 64), posf[s:s + 1], ALU.mult)
    ts(kk.all(), ang.all(), 1.0 / TWO_PI, ALU.mult, MAGIC, ALU.add)
    ts(kk.all(), kk.all(), -MAGIC, ALU.add)
    stt(ang.all(), kk.all(), -C1, ang.all(), ALU.mult, ALU.add)
    stt(ang.all(), kk.all(), -C2, ang.all(), ALU.mult, ALU.add)
    ts(kk.all(), ang.all(), math.pi / 2, ALU.is_gt, -TWO_PI, ALU.mult)
    stt(kk.all(), ang.all(), math.pi / 2, kk.all(), ALU.add, ALU.add)
    ts(ang.all(), ang.all(), -3.141592, ALU.max, 3.141592, ALU.min)
    ts(kk.all(), kk.all(), -3.141592, ALU.max, 3.141592, ALU.min)
    act(sinT.all(), ang.all(), AF.Sin)
    act(cosT.all(), kk.all(), AF.Sin)

    def w2d(dr, l, K, C, r0, c0, nk, ncol):
        return dr.v(l * K * C + r0 * C + c0, [(C, 128), (128 * C, nk), (1, ncol)])

    def blocks_for(l):
        B = {}

        def colblk(dr, K, C, c0):
            return [(None, w2d(dr, l, K, C, 0, c0, 8, 512))]
        B[0] = colblk(w_in, D, IN_COLS, OFF_Q)
        B[1] = colblk(w_in, D, IN_COLS, OFF_K)
        B[2] = colblk(w_in, D, IN_COLS, OFF_V)
        B[3] = colblk(w_in, D, IN_COLS, OFF_V + 512)
        B[4] = colblk(w_in, D, IN_COLS, OFF_G)
        B[5] = colblk(w_in, D, IN_COLS, OFF_G + 512)
        B[6] = colblk(w_in, D, IN_COLS, OFF_POOL)
        B[7] = colblk(w_in, D, IN_COLS, OFF_XQ)
        for hf in range(2):
            for b in range(3):
                B[8 + 5 * hf + b] = colblk(w_in, D, IN_COLS, OFF_GATE + b * 1024 + hf * 512)
            B[11 + 5 * hf] = [((0, 4), w2d(w_up_pool, l, 512, D, 0, hf * 512, 4, 512)),
                              ((4, 4), w2d(w_up_mem, l, 512, D, 0, hf * 512, 4, 512))]
            B[12 + 5 * hf] = colblk(w_up_ret, 1024, D, hf * 512)
            B[18 + hf] = colblk(w_out, D, D, hf * 512)
        for b in range(8):
            B[20 + b] = colblk(w_mlp1, D, 4096, b * 512)
        for hf in range(2):
            for fb in range(4):
                B[28 + hf * 4 + fb] = [(None, w2d(w_mlp2, l, 4096, D, fb * 1024, hf * 512, 8, 512))]
        B[36] = colblk(w_mem_kv, D, 1024, 0)
        B[37] = colblk(w_mem_kv, D, 1024, 512)
        return B

    def wq_blk(l, b):
        return wq.v((l * NBLK + b) * 128 * 4096, [(4096, 128), (512, 8), (1, 512)])

    def cast_blocks(l, ids):
        B = blocks_for(l)
        for b in ids:
            cast_done.add((l, b))
            for part, src in B[b]:
                base = (l * NBLK + b) * 128 * 4096
                if part is None:
                    dst = wq.v(base, [(4096, 128), (512, 8), (1, 512)])
                else:
                    dst = wq.v(base + part[0] * 512, [(4096, 128), (512, part[1]), (1, 512)])
                dma(dst, src, eng=POOL)

    PRE_IDS = [1, 2, 3, 6]
    ALL_IDS = [36, 37] + list(range(36))
    REST_IDS = [b for b in ALL_IDS if b not in PRE_IDS]
    castq = []
    cast_done = set()

    def pump(n):
        for _ in range(min(n, len(castq))):
            l, b = castq.pop(0)
            cast_blocks(l, [b])

    state = {"ring": 0, "ps": 0}

    def ring_load(l, b):
        assert (l, b) in cast_done, (l, b)
        slot = ring[state["ring"] % RING]
        state["ring"] += 1
        dma(A16.v(slot.off, [(512, 8), (1, 512)]), wq_blk(l, b))
        return slot

    PSROT = [PS[0], PS[1], PS[3], PS[4], PS[5]]

    def ps_next():
        a = PSROT[state["ps"] % len(PSROT)]
        state["ps"] += 1
        return a

    gvec = lambda base, l: Tl(A32, VEC.off + base + l * 8, [8])
    pscale = lambda l: Tl(A32, VEC.off + V_PSC + l * 4, [4])

    def rms_rstd(src_rows, n, dim):
        for i, v in enumerate(src_rows):
            act(junk[0:dim], v, AF.Square, accum=ss[i:i + 1])
        ts(rstd[0:n], ss[0:n], 1.0 / dim, ALU.mult, EPS, ALU.add, eng=POOL)
        tt(rstd[0:n], rstd[0:n], cv(c_neghalf, n), ALU.pow, eng=POOL)

    def norm_to_hT(gv):
        rms_rstd([X[st] for st in range(NST)], NST, D)
        for st in range(NST):
            ts(hn[st], X[st], rstd[st:st + 1], ALU.mult)
        for kc in range(8):
            pb = PB[kc % 2]
            for st in range(NST):
                tp(pb.v(st * 128, [(1, 128)]), hn[st, kc * 128:(kc + 1) * 128])
            if kc % 2:
                ts(hT[kc], pb.v(0, [(1, 512)]), gv[kc:kc + 1], ALU.mult)
            else:
                act(hT[kc], pb.v(0, [(1, 512)]), AF.Copy, scale=gv[kc:kc + 1])

    def load_x(src, t, buf):
        for st in range(NST):
            dma(buf[st], src.v((t * T + st * 128) * D, [(D, 128), (1, D)]))

    def proj_tok(blk, st, evac):
        ps = ps_next()
        for kc in range(8):
            mm(ps.v(0, [(1, 512)]), hT[kc, st * 128:(st + 1) * 128], blk[kc], start=(kc == 0), stop=(kc == 7))
        evac(ps)

    def proj_feat(blk, j, evac, nk=8, rhs=None, kofs=0):
        ps = ps_next()
        rhs = rhs or hT
        for kc in range(nk):
            mm(ps.v(0, [(1, 512)]), blk[kofs + kc, j * 128:(j + 1) * 128], rhs[kc], start=(kc == 0), stop=(kc == nk - 1))
        evac(ps)

    def rotary(ps, gst, dst, kscale):
        src4 = ps.v(0, [(64, 8), (1, 64)])
        cosb = A32.v(cosT.off + gst * 64, [(0, 8), (1, 64)])
        sinb = A32.v(sinT.off + gst * 64, [(0, 8), (1, 64)])
        tt(A32.v(rotA.off, [(64, 8), (1, 64)]), src4, cosb, ALU.mult)
        tt(A32.v(rotB.off, [(64, 8), (1, 64)]), src4, sinb, ALU.mult)
        a1 = A32.v(rotA.off, [(128, 4), (1, 64)])
        a2 = A32.v(rotA.off + 64, [(128, 4), (1, 64)])
        b1 = A32.v(rotB.off, [(128, 4), (1, 64)])
        b2 = A32.v(rotB.off + 64, [(128, 4), (1, 64)])
        if kscale:
            o1 = A32.v(rotR.off, [(128, 4), (1, 64)])
            o2 = A32.v(rotR.off + 64, [(128, 4), (1, 64)])
        else:
            o1 = A16.v(dst.off, [(128, 4), (1, 64)])
            o2 = A16.v(dst.off + 64, [(128, 4), (1, 64)])
        tt(o1, a1, b2, ALU.subtract, eng=POOL)
        tt(o2, a2, b1, ALU.add, eng=POOL)
        if kscale:
            tt(dst.all(), rotR.all(), cv(c_kscale, 512), ALU.mult, eng=POOL)

    def sub(tl, i):
        return Tl(tl.a, tl.off + i * tl.strides[0], tl.shape[1:])

    def kv_update(c, chunk_idx):
        for hp in range(2):
            ps = ps_next()
            for hh in range(2):
                h = hp * 2 + hh
                mm(ps.v(hh * 256, [(1, 256)]), kpr[c, h * 128:(h + 1) * 128], vv[c, h * 256:(h + 1) * 256])
            for hh in range(2):
                h = hp * 2 + hh
                stt(Z[h], Z[h], GC[h], ps.v(hh * 256, [(1, 256)]), ALU.mult, ALU.add)

    def refresh_Sb():
        for h in range(H):
            act(Sb[h], Z[h], AF.Copy, scale=GC[h])

    def exchange_out(l, last_U):
        cp(exl[0:1024], Z.all(), eng=POOL)
        cp(A32.v(exl.off + 1024, [(16, 4), (1, 16)]), A32.v(U.off + 512, [(528, 4), (1, 16)]), eng=POOL)
        dma(exs[l].v(0, [(EXN, 128), (1, EXN)]), exl.all())

    def exchange_in(l):
        if fused:
            S.add(POOL, lambda e: e.collective_compute("AllGather", ALU.bypass, [[0, 1], [2, 3], [4, 5], [6, 7]],
                                                       ins=[exs[l].v(0, [(EXN, 128), (1, EXN)]).ap],
                                                       outs=[exg[l].v(0, [(EXN, 256), (1, EXN)]).ap]),
                  [exs[l].v(0, [(EXN, 128), (1, EXN)])], [exg[l].v(0, [(EXN, 256), (1, EXN)])], cc=True)
        dma(exl.all(), exg[l].v(0, [(EXN, 128), (1, EXN)]))
        ts(Z.all(), exl[0:1024], cv(c_isB, 1), ALU.mult)
        ts(halo.all(), exl[1024:1088], cv(c_isB, 1), ALU.mult)

    def mem_pre(l):
        for c in range(2):
            dma(Xb[0][c], mem_in.v(c * 128 * D, [(D, 128), (1, D)]))
        rms_rstd([Xb[0][c] for c in range(2)], 2, D)
        for c in range(2):
            ts(memn[c], Xb[0][c], rstd[c:c + 1], ALU.mult)
        gv = gvec(V_GMEM, l)
        for kc in range(8):
            pb = PB[kc % 2]
            for c in range(2):
                tp(pb.v(c * 128, [(1, 128)]), memn[c, kc * 128:(kc + 1) * 128])
            ts(memnT[kc], pb.v(0, [(1, 256)]), gv[kc:kc + 1], ALU.mult)
        bk = ring_load(l, 36)
        for h in range(H):
            ps = ps_next()
            for kc in range(8):
                mm(ps.v(0, [(1, 256)]), bk[kc, h * 128:(h + 1) * 128], memnT[kc], start=(kc == 0), stop=(kc == 7))
            act(kmT[h], ps.v(0, [(1, 256)]), AF.Copy)
        bv = ring_load(l, 37)
        for c in range(2):
            ps = ps_next()
            for kc in range(8):
                mm(ps.v(0, [(1, 512)]), memnT[kc, c * 128:(c + 1) * 128], bv[kc], start=(kc == 0), stop=(kc == 7))
            act(vm[c], ps.v(0, [(1, 512)]), AF.Copy)

    def prepass(l, src):
        mset(Z.all(), 0.0)
        gv = gvec(V_GMIX, l)
        nxt = None
        for t in range(NT):
            pump(5)
            if t == 0:
                load_x(src, t, X.flip())
            else:
                X.flip()
            norm_to_hT(gv)
            if t + 1 < NT:
                load_x(src, t + 1, Xb[(X.n + 1) % 2])
            bk = ring_load(l, 1)
            for st in range(NST):
                proj_tok(bk, st, lambda ps, st=st: rotary(ps, t * NST + st, sub(kpr, st), True))
            for hf in range(2):
                bv = ring_load(l, 2 + hf)
                for st in range(NST):
                    proj_tok(bv, st, lambda ps, st=st, hf=hf: act(vv[st, hf * 512:(hf + 1) * 512], ps.v(0, [(1, 512)]), AF.Copy))
            for c in range(NST):
                kv_update(c, t * NST + c)
            if t == NT - 1:
                bp = ring_load(l, 6)
                for g in range(4):
                    proj_feat(bp, g, lambda ps, g=g: act(U[g, 16:528], ps.v(0, [(1, 512)]), AF.Copy))
        exchange_out(l, None)

    def main(l, src, dst_x, final):
        gv = gvec(V_GMIX, l)
        gv2 = gvec(V_GMLP, l)
        dma(retg.all(), rows.v(R_RETG + l * 1024, [(0, 128), (1, 1024)]))
        dma(A16.v(poolw.off, [(128, 4), (1, 128)]), pool_w.v(l * 4 * 128 * 128, [(128, 128), (128 * 128, 4), (1, 128)]), eng=POOL)
        for t in range(NT):
            pump(5)
            if t == 0:
                load_x(src, t, X.flip())
            else:
                X.flip()
            norm_to_hT(gv)
            if t + 1 < NT:
                load_x(src, t + 1, Xb[(X.n + 1) % 2])
            bq = ring_load(l, 0)
            for st in range(NST):
                proj_tok(bq, st, lambda ps, st=st: rotary(ps, t * NST + st, sub(qrot, st), False))
            bk = ring_load(l, 1)
            for st in range(NST):
                proj_tok(bk, st, lambda ps, st=st: rotary(ps, t * NST + st, sub(kpr, st), True))
            for hf in range(2):
                bv = ring_load(l, 2 + hf)
                for st in range(NST):
                    proj_tok(bv, st, lambda ps, st=st, hf=hf: act(vv[st, hf * 512:(hf + 1) * 512], ps.v(0, [(1, 512)]), AF.Copy))
            for hf in range(2):
                bg = ring_load(l, 4 + hf)
                for st in range(NST):
                    def ev(ps, st=st, hf=hf):
                        act(gs[st, hf * 512:(hf + 1) * 512], ps.v(0, [(1, 512)]), AF.Silu)
                        tt(gs[st, hf * 512:(hf + 1) * 512], gs[st, hf * 512:(hf + 1) * 512],
                           retg[hf * 512:(hf + 1) * 512], ALU.mult, eng=POOL)
                    proj_tok(bg, st, ev)
            chk("proj", [(qrot, 512), (kpr, 512), (vv, 1024), (gs, 1024)])
            for c in range(NST):
                par = c % 2
                refresh_Sb()
                for h in range(H):
                    tp(PB[0].v(h * 128, [(1, 128)]), qrot[c, h * 128:(h + 1) * 128])
                    tp(PB[1].v(h * 128, [(1, 128)]), kpr[c, h * 128:(h + 1) * 128])
                act(qT[par].all(), PB[0].v(0, [(1, 512)]), AF.Copy)
                cp(kT[par].all(), PB[1].v(0, [(1, 512)]))
                chk("r1", [(qT[0], 512), (kT[0], 512), (Sb, 1024)])
                for h in range(H):
                    mm(PS[2].v(h * 128, [(1, 128)]), kT[par][h], qT[par][h])
                tt(scm[par].all(), PS[2].v(0, [(1, 512)]), cv(c_cmask, 512), ALU.mult)
                chk("r2", [(scm[0], 512)])
                ysb = Tl(A32, rotA.off, [4, 256])
                for h in range(H):
                    ps = ps_next()
                    mm(ps.v(0, [(1, 256)]), scm[par][h], vv[c, h * 256:(h + 1) * 256])
                    mm(ps.v(256, [(1, 256)]), qT[par][h], Sb[h])
                    act(ysb[h], ps.v(0, [(1, 256)]), AF.Copy)
                    chk("r2x", [(ysb, 256)])
                    stt(ysb[h], ysb[h], 1.0, ps.v(256, [(1, 256)]), ALU.mult, ALU.add)
                    chk("r2y", [(ysb, 256)])
                chk("r2a", [(ysb, 1024)])
                for h in range(H):
                    act(junk[0:256], ysb[h], AF.Square, accum=ys[h:h + 1])
                chk("r2b", [(ysb, 1024), (ys, 8)])
                tt(ys[4:8], ys[0:4], cv(c_yscale2, 4), ALU.mult, eng=POOL)
                ts(ys[4:8], ys[4:8], 1.0, ALU.mult, EPS, ALU.add, eng=POOL)
                tt(ys[4:8], ys[4:8], cv(c_neghalf, 4), ALU.pow, eng=POOL)
                tt(sc[0:4], ys[4:8], cv(c_yscale, 4), ALU.mult, eng=POOL)
                for h in range(H):
                    ts(ysb[h], ysb[h], sc[h:h + 1], ALU.mult)
                    tt(yret[par][h * 256:(h + 1) * 256], ysb[h], gs[c, h * 256:(h + 1) * 256], ALU.mult, eng=POOL)
                chk("r3", [(yret[0], 1024)])
                kv_update(c, t * NST + c)
                chk("r4", [(yret[0], 1024)])
                pb = PB[par]
                for kc in range(8):
                    tp(pb.v(kc * 128, [(1, 128)]), yret[par][kc * 128:(kc + 1) * 128])
                act(A16.v(yretT.off + c * 128, [(512, 8), (1, 128)]), pb.v(0, [(128, 8), (1, 128)]), AF.Copy)
            chk("ret", [(yretT, 4096)])
            cp(A32.v(U.off, [(528, 4), (1, 16)]), A32.v(halo.off, [(16, 4), (1, 16)]), eng=POOL)
            bp = ring_load(l, 6)
            for g in range(4):
                proj_feat(bp, g, lambda ps, g=g: act(U[g, 16:528], ps.v(0, [(1, 512)]), AF.Copy))
            cp(A32.v(halo.off, [(16, 4), (1, 16)]), A32.v(U.off + 512, [(528, 4), (1, 16)]), eng=POOL)
            for g in range(4):
                w = 2 << g
                cur, curlen, shift = sub(U, g), 528, 1
                bufs = [tmpA, tmpB]
                bi = 0
                while shift < w:
                    nl = curlen - shift
                    nxt = bufs[bi]
                    tt(nxt[0:nl], cur[shift:curlen], cur[0:nl], ALU.add, eng=POOL)
                    cur, curlen = nxt, nl
                    bi ^= 1
                    shift *= 2
                o = 17 - w
                stt(dd[g], cur[o:o + 512], 1.0 / w, U[g, 16:528], ALU.mult, ALU.subtract)
                if t == 0:
                    tt(mtmp[0, 0:16], cur[o:o + 16], cv(c_invcnt + g * 16, 16), ALU.mult)
                    tt(dd[g, 0:16], mtmp[0, 0:16], U[g, 16:32], ALU.subtract)
            psc = pscale(l)
            for g in range(4):
                ps = ps_next()
                mm(ps.v(0, [(1, 512)]), poolw[g], dd[g])
                act(ypoolT[g], ps.v(0, [(1, 512)]), AF.Copy, scale=psc[g:g + 1])
            chk("pool", [(ypoolT, 2048)])
            bx = ring_load(l, 7)
            for h in range(H):
                proj_feat(bx, h, lambda ps, h=h: act(xqT[h], ps.v(0, [(1, 512)]), AF.Copy))
            for h in range(H):
                par = h % 2
                for c in range(2):
                    ps = ps_next()
                    mm(ps.v(0, [(1, 512)]), kmT[h, c * 128:(c + 1) * 128], xqT[h])
                    act(pT[par][c], ps.v(0, [(1, 512)]), AF.Exp, scale=128 ** -0.5)
                pso = ps_next()
                for c in range(2):
                    mm(pso.v(0, [(1, 512)]), vm[c, h * 128:(h + 1) * 128], pT[par][c], start=(c == 0), stop=(c == 1))
                for c in range(2):
                    mm(PS[2].v(0, [(1, 512)]), ones.all(), pT[par][c], start=(c == 0), stop=(c == 1))
                recip(rsum.all(), PS[2].v(0, [(1, 512)]))
                tt(ymemT[h], pso.v(0, [(1, 512)]), rsum.all(), ALU.mult)
            chk("mem", [(ymemT, 2048)])
            for hf in range(2):
                for b in range(3):
                    bgate = ring_load(l, 8 + 5 * hf + b)
                    for j in range(4):
                        proj_feat(bgate, j, lambda ps, b=b, j=j: act(sg[b, j], ps.v(0, [(1, 512)]), AF.Sigmoid))
                bpm = ring_load(l, 11 + 5 * hf)
                bret = ring_load(l, 12 + 5 * hf)
                for j in range(4):
                    proj_feat(bpm, j, lambda ps, j=j: tt(mtmp[0], ps.v(0, [(1, 512)]), sg[0, j], ALU.mult), nk=4, rhs=ypoolT)
                    proj_feat(bret, j, lambda ps, j=j: tt(mtmp[1], ps.v(0, [(1, 512)]), sg[1, j], ALU.mult), nk=8, rhs=yretT)
                    tt(mtmp[0], mtmp[0], mtmp[1], ALU.add, eng=POOL)
                    proj_feat(bpm, j, lambda ps, j=j: tt(mtmp[1], ps.v(0, [(1, 512)]), sg[2, j], ALU.mult), nk=4, rhs=ymemT, kofs=4)
                    tt(mergedT[hf * 4 + j], mtmp[0], mtmp[1], ALU.add, eng=POOL)
            chk("merge", [(mergedT, 4096)])
            for hf in range(2):
                bo = ring_load(l, 18 + hf)
                for st in range(NST):
                    ps = ps_next()
                    for kc in range(8):
                        mm(ps.v(0, [(1, 512)]), mergedT[kc, st * 128:(st + 1) * 128], bo[kc], start=(kc == 0), stop=(kc == 7))
                    tt(X[st, hf * 512:(hf + 1) * 512], ps.v(0, [(1, 512)]), X[st, hf * 512:(hf + 1) * 512], ALU.add)
            chk("out", [(X.cur, 4096)])
            norm_to_hT(gv2)
            for b in range(8):
                b1 = ring_load(l, 20 + b)
                for j in range(4):
                    def ev(ps, b=b, j=j):
                        act(mtmp[j % 2], ps.v(0, [(1, 512)]), AF.Relu)
                        tt(hid[b * 4 + j], mtmp[j % 2], mtmp[j % 2], ALU.mult, eng=POOL)
                    proj_feat(b1, j, ev)
            for hf in range(2):
                b2s = [ring_load(l, 28 + hf * 4 + fb) for fb in range(4)]
                for st in range(NST):
                    ps = ps_next()
                    for fb in range(4):
                        for k8 in range(8):
                            mm(ps.v(0, [(1, 512)]), hid[fb * 8 + k8, st * 128:(st + 1) * 128], b2s[fb][k8],
                               start=(fb == 0 and k8 == 0), stop=(fb == 3 and k8 == 7))
                    tt(X[st, hf * 512:(hf + 1) * 512], ps.v(0, [(1, 512)]), X[st, hf * 512:(hf + 1) * 512], ALU.add)
            chk("mlp", [(X.cur, 4096)])
            if final:
                rms_rstd([X[st] for st in range(NST)], NST, D)
                for st in range(NST):
                    stt(X[st], X[st], rstd[st:st + 1], fing.all(), ALU.mult, ALU.mult)
            for st in range(NST):
                dma(dst_x.v((t * T + st * 128) * D, [(D, 128), (1, D)]), X[st])

    def _program():
        if do_pre0 and not fused:
            cast_blocks(0, PRE_IDS)
            prepass(0, x_in)
        if fused:
            cast_blocks(0, PRE_IDS)
            castq.extend([(0, b) for b in REST_IDS] + [(1, b) for b in PRE_IDS] + [(1, b) for b in REST_IDS])
            prepass(0, x_in)
            pump(len(REST_IDS) + len(PRE_IDS) - 40 if len(REST_IDS) + len(PRE_IDS) > 40 else 0)
            while castq and castq[0][0] == 0:
                pump(1)
            exchange_in(0)
            mem_pre(0)
            main(0, x_in, xs, False)
            while castq and castq[0] in [(1, b) for b in PRE_IDS]:
                pump(1)
            prepass(1, xs)
            pump(len(castq))
            exchange_in(1)
            mem_pre(1)
            main(1, xs, out_d, True)
            return
        if do_main0:
            cast_blocks(0, ALL_IDS)
            cast_blocks(1, PRE_IDS)
            exchange_in(0)
            mem_pre(0)
            main(0, x_in, xs, False)
            prepass(1, xs)
        if do_main1:
            cast_blocks(1, ALL_IDS)
            exchange_in(1)
            mem_pre(1)
            main(1, xs, out_d, True)

    try:
        _program()
    except _Stop:
        pass
    S.emit(nc)
    return nc


def _consts(core):
    half = core % 2
    c = np.zeros((128, CST_N), np.float32)
    j = np.arange(128)
    c[:, c_ident:c_ident + 128] = np.eye(128, dtype=np.float32)
    cm = (j[None, :] >= j[:, None]).astype(np.float32)
    c[:, c_cmask:c_cmask + 512] = np.tile(cm, (1, 4))
    for h in range(H):
        g = np.float64(GAMMA[h])
        c[:, c_kscale + h * 128:c_kscale + (h + 1) * 128] = (g ** (-(j + 1.0)) * 128 ** -0.5)[:, None]
        c[:, c_yscale + h] = g ** (j + 1.0)
        c[:, c_yscale2 + h] = g ** (2.0 * (j + 1.0)) / 256.0
        c[:, c_gcb + h] = g ** 128 * half
    c[:, c_invf:c_invf + 64] = (10000.0 ** (-np.arange(64, dtype=np.float64) / 64.0)).astype(np.float32)[None, :]
    for gi, w in enumerate((2, 4, 8, 16)):
        tt_ = np.arange(16)
        cnt = np.minimum(tt_ + 1, w) if half == 0 else np.full(16, w)
        c[:, c_invcnt + gi * 16:c_invcnt + (gi + 1) * 16] = (1.0 / cnt)[None, :]
    c[:, c_isB] = float(half)
    c[:, c_neghalf:c_neghalf + 8] = -0.5
    return c


def _in_map(core, inp):
    b, half = core // 2, core % 2
    sl = slice(half * TOK, (half + 1) * TOK)
    f = lambda a: np.ascontiguousarray(a, dtype=np.float32)
    vec = np.zeros((128, VEC_N), np.float32)
    for l in range(2):
        vec[:, V_GMIX + l * 8:V_GMIX + l * 8 + 8] = inp["norm_mix_g"][l].reshape(8, 128).T
        vec[:, V_GMLP + l * 8:V_GMLP + l * 8 + 8] = inp["norm_mlp_g"][l].reshape(8, 128).T
        vec[:, V_GMEM + l * 8:V_GMEM + l * 8 + 8] = inp["mem_norm_g"][l].reshape(8, 128).T
        vec[:, V_PSC + l * 4:V_PSC + l * 4 + 4] = inp["pool_scale"][l].reshape(4, 128).T
    rows = np.concatenate([inp["ret_norm_g"][0], inp["ret_norm_g"][1], inp["final_norm_g"]]).astype(np.float32)
    return {
        "x": f(inp["x"][b, sl]),
        "mem": f(inp["mem"][b]),
        "posT": np.ascontiguousarray(np.asarray(inp["positions"])[b, sl].reshape(32, 128).T.astype(np.int32)),
        "cst": _consts(core),
        "vecs": vec,
        "rows": rows,
        "w_in": f(inp["w_in"]), "pool_w": f(inp["pool_w"]), "w_mem_kv": f(inp["w_mem_kv"]),
        "w_up_pool": f(inp["w_up_pool"]), "w_up_ret": f(inp["w_up_ret"]), "w_up_mem": f(inp["w_up_mem"]),
        "w_out": f(inp["w_out"]), "w_mlp1": f(inp["w_mlp1"]), "w_mlp2": f(inp["w_mlp2"]),
    }


FUSED = True


def kernel(**inputs):
    inp = {k: np.asarray(v) for k, v in inputs.items()}
    maps = [_in_map(c, inp) for c in range(NCORES)]
    cores = list(range(NCORES))
    if FUSED:
        nc = build("fused")
        res = run_bass_kernel_spmd(nc, maps, core_ids=cores)
        outs = [r["out"] for r in res.results]
    else:
        def partner(arrs):
            return [arrs[c - 1] if c % 2 else arrs[c] for c in range(NCORES)]
        r0 = run_bass_kernel_spmd(build("unfused", 0), maps, core_ids=cores).results
        ex0 = partner([r["exs0"] for r in r0])
        m1 = [dict(m, exg0=ex0[c]) for c, m in enumerate(maps)]
        r1 = run_bass_kernel_spmd(build("unfused", 1), m1, core_ids=cores).results
        ex1 = partner([r["exs1"] for r in r1])
        m2 = [dict(m, exg1=ex1[c], xs=r1[c]["xs"]) for c, m in enumerate(maps)]
        r2 = run_bass_kernel_spmd(build("unfused", 2), m2, core_ids=cores).results
        outs = [r["out"] for r in r2]
    out = np.empty((4, 8192, D), np.float32)
    for c in range(NCORES):
        out[c // 2, (c % 2) * TOK:(c % 2 + 1) * TOK] = outs[c]
    return out
```
